# Optimizing a Trainium2 kernel written in Bass

```python
import jax, jax.numpy as jnp
from jax import lax
import numpy as np

D_MODEL = 1024
BATCH = 8
SEQ = 4096
DEPTH = 1

D_MIX = D_MODEL
D_GMLP = D_MIX // 2
GMLP_GROUPS = 4
GMLP_GROUP_DIM = D_GMLP // GMLP_GROUPS
CHUNK = 128
D_SB = D_MIX - D_GMLP
SB_HEADS = 8
SB_HEAD_DIM = D_SB // SB_HEADS
Q_BLOCK = 128
D_IN = 2 * D_GMLP + 3 * D_SB
N_EXPERTS = 32
TOP_K = 4
D_FF = D_MODEL
SWIGLU_LIMIT = 7.0
SWIGLU_ALPHA = 1.702
MOE_BLOCK = 512
EPS = 1e-5

kernel_name = "hymba_gmlp_stickbreaking_moe_block"


def rms_norm(x, w):
    xf = x.astype(jnp.float32)
    y = xf * lax.rsqrt(jnp.mean(xf * xf, axis=-1, keepdims=True) + EPS)
    return (y * w.astype(jnp.float32)).astype(x.dtype)


def group_rms_norm(x, w, groups):
    shp = x.shape
    xg = x.reshape(shp[:-1] + (groups, shp[-1] // groups))
    return rms_norm(xg, w.reshape(groups, -1)).reshape(shp)


def chunked_spatial_gating(uv, sgu_norm_w, sgu_w, sgu_b):
    B, S, _ = uv.shape
    z = jax.nn.gelu(uv)
    u, v = jnp.split(z, 2, axis=-1)
    v = group_rms_norm(v, sgu_norm_w, GMLP_GROUPS)
    v = v.reshape(B, S // CHUNK, CHUNK, GMLP_GROUPS, GMLP_GROUP_DIM)
    causal = jnp.tril(jnp.ones((CHUNK, CHUNK), sgu_w.dtype))
    w = sgu_w * causal[None]
    mixed = jnp.einsum("gts,bnsgc->bntgc", w, v) + sgu_b.T[:, :, None]
    return u * mixed.reshape(B, S, D_GMLP)


def stick_breaking_attention(q, k, v, q_norm_w, k_norm_w):
    B, S, _ = q.shape

    def heads(t):
        return t.reshape(B, S, SB_HEADS, SB_HEAD_DIM)

    qh = rms_norm(heads(q), q_norm_w).transpose(0, 2, 1, 3)
    kh = rms_norm(heads(k), k_norm_w).transpose(0, 2, 1, 3)
    vh = heads(v).transpose(0, 2, 1, 3)
    scale = SB_HEAD_DIM ** -0.5
    outs = []
    for i in range(S // Q_BLOCK):
        L = (i + 1) * Q_BLOCK
        qb = qh[:, :, i * Q_BLOCK:L]
        z = jnp.einsum("bhtd,bhsd->bhts", qb, kh[:, :, :L]).astype(jnp.float32) * scale
        t_pos = i * Q_BLOCK + jnp.arange(Q_BLOCK)
        s_pos = jnp.arange(L)
        causal = s_pos[None, :] < t_pos[:, None]
        log_beta = jax.nn.log_sigmoid(z)
        log_1m_beta = jnp.where(causal, jax.nn.log_sigmoid(-z), 0.0)
        survive = lax.cumsum(log_1m_beta, axis=log_1m_beta.ndim - 1, reverse=True) - log_1m_beta
        a = jnp.where(causal, jnp.exp(log_beta + survive), 0.0)
        outs.append(jnp.einsum("bhts,bhsd->bhtd", a.astype(vh.dtype), vh[:, :, :L]))
    o = jnp.concatenate(outs, axis=2)
    return o.transpose(0, 2, 1, 3).reshape(B, S, D_SB)


def moe(h, router_w, router_b, w_gate_up, b_gate_up, w_down, b_down):
    B, S, D = h.shape
    xt = h.reshape(-1, D)
    N = xt.shape[0]
    logits = xt.astype(jnp.float32) @ router_w.astype(jnp.float32) + router_b.astype(jnp.float32)
    top_logits, top_idx = lax.top_k(logits, TOP_K)
    gates = jax.nn.softmax(top_logits, axis=-1)
    n_assign = N * TOP_K
    flat_e = top_idx.reshape(-1)
    flat_tok = jnp.arange(n_assign, dtype=jnp.int32) // TOP_K
    order = jnp.argsort(flat_e)
    sorted_e = flat_e[order]
    counts = jnp.bincount(flat_e, length=N_EXPERTS)
    start = jnp.cumsum(counts) - counts
    padded = (counts + MOE_BLOCK - 1) // MOE_BLOCK * MOE_BLOCK
    pad_end = jnp.cumsum(padded)
    pad_start = pad_end - padded
    dest = pad_start[sorted_e] + (jnp.arange(n_assign) - start[sorted_e])
    n_blocks = -(-n_assign // MOE_BLOCK) + N_EXPERTS
    P = n_blocks * MOE_BLOCK
    slot_tok = jnp.zeros((P,), jnp.int32).at[dest].set(flat_tok[order])
    slot_w = jnp.zeros((P,), gates.dtype).at[dest].set(gates.reshape(-1)[order])
    block_start = jnp.arange(n_blocks) * MOE_BLOCK
    block_expert = jnp.minimum(jnp.searchsorted(pad_end, block_start, side="right"), N_EXPERTS - 1)

    def expert_block(args):
        tok, e = args
        xb = xt[tok]
        gu = xb @ w_gate_up[e] + b_gate_up[e]
        gate = jnp.minimum(gu[:, 0::2], SWIGLU_LIMIT)
        up = jnp.clip(gu[:, 1::2], -SWIGLU_LIMIT, SWIGLU_LIMIT)
        glu = gate * jax.nn.sigmoid(SWIGLU_ALPHA * gate)
        return ((up + 1.0) * glu) @ w_down[e] + b_down[e]

    y_slots = lax.map(expert_block, (slot_tok.reshape(n_blocks, MOE_BLOCK), block_expert))
    y_slots = y_slots.reshape(P, D).astype(jnp.float32) * slot_w[:, None]
    y = jax.ops.segment_sum(y_slots, slot_tok, num_segments=N)
    return y.reshape(B, S, D).astype(h.dtype)


def setup_inputs(seed: int = 0) -> dict:
    key = jax.random.key(seed)
    ks = jax.random.split(key, 18)
    f32 = jnp.float32

    def nrm(k, shape, scale):
        return jax.random.normal(k, shape, f32) * scale

    def gain(k, shape):
        return 1.0 + 0.05 * jax.random.normal(k, shape, f32)

    return {
        "x": jax.random.normal(ks[0], (BATCH, SEQ, D_MODEL), f32),
        "norm1_w": gain(ks[1], (DEPTH, D_MODEL)),
        "w_in": nrm(ks[2], (DEPTH, D_MODEL, D_IN), D_MODEL ** -0.5),
        "sgu_norm_w": gain(ks[3], (DEPTH, D_GMLP)),
        "sgu_w": nrm(ks[4], (DEPTH, GMLP_GROUPS, CHUNK, CHUNK), 0.5 * CHUNK ** -0.5),
        "sgu_b": gain(ks[5], (DEPTH, GMLP_GROUPS, CHUNK)),
        "q_norm_w": gain(ks[6], (DEPTH, SB_HEAD_DIM)),
        "k_norm_w": gain(ks[7], (DEPTH, SB_HEAD_DIM)),
        "out_norm_a_w": gain(ks[8], (DEPTH, D_GMLP)),
        "out_norm_b_w": gain(ks[9], (DEPTH, D_SB)),
        "w_out": nrm(ks[10], (DEPTH, D_MIX, D_MODEL), D_MIX ** -0.5),
        "norm2_w": gain(ks[11], (DEPTH, D_MODEL)),
        "router_w": nrm(ks[12], (DEPTH, D_MODEL, N_EXPERTS), D_MODEL ** -0.5),
        "router_b": nrm(ks[13], (DEPTH, N_EXPERTS), 0.01),
        "w_gate_up": nrm(ks[14], (DEPTH, N_EXPERTS, D_MODEL, 2 * D_FF), D_MODEL ** -0.5),
        "b_gate_up": nrm(ks[15], (DEPTH, N_EXPERTS, 2 * D_FF), 0.01),
        "w_down": nrm(ks[16], (DEPTH, N_EXPERTS, D_FF, D_MODEL), D_FF ** -0.5),
        "b_down": nrm(ks[17], (DEPTH, N_EXPERTS, D_MODEL), 0.01),
    }


def reference(x, norm1_w, w_in, sgu_norm_w, sgu_w, sgu_b, q_norm_w, k_norm_w,
              out_norm_a_w, out_norm_b_w, w_out, norm2_w, router_w, router_b,
              w_gate_up, b_gate_up, w_down, b_down):
    for l in range(DEPTH):
        h = rms_norm(x, norm1_w[l])
        proj = h @ w_in[l]
        uv, q, k, v = jnp.split(proj, [2 * D_GMLP, 2 * D_GMLP + D_SB, 2 * D_GMLP + 2 * D_SB], axis=-1)
        y_a = chunked_spatial_gating(uv, sgu_norm_w[l], sgu_w[l], sgu_b[l])
        y_b = stick_breaking_attention(q, k, v, q_norm_w[l], k_norm_w[l])
        y_a = group_rms_norm(y_a, out_norm_a_w[l], GMLP_GROUPS)
        y_b = group_rms_norm(y_b, out_norm_b_w[l], SB_HEADS)
        x = x + jnp.concatenate([y_a, y_b], axis=-1) @ w_out[l]
        x = x + moe(rms_norm(x, norm2_w[l]), router_w[l], router_b[l],
                    w_gate_up[l], b_gate_up[l], w_down[l], b_down[l])
    return x
```

```python
import os
import numpy as np
from contextlib import ExitStack
import concourse.bass as bass
import concourse.mybir as mybir
from concourse.bass_utils import run_bass_kernel_spmd

F32 = mybir.dt.float32
BF16 = mybir.dt.bfloat16
I32 = mybir.dt.int32
AF = mybir.ActivationFunctionType
ALU = mybir.AluOpType
AX = mybir.AxisListType

S_TOK = 4096
D = 1024
NT = S_TOK // 128
NE = 32
NBLK = 64
BLK = 512
NSLOT = NBLK * BLK
EPS = 1e-5

ENGS = ("pe", "act", "dve", "pool", "sp")
N_DMA_SEMS = 48


class Buf:
    __slots__ = ("name", "last_w", "readers")

    def __init__(self, name=""):
        self.name = name
        self.last_w = None
        self.readers = []


class Op:
    __slots__ = ("eng", "fn", "idx", "waits", "signal", "count", "vc", "dma", "dsem", "dval", "region")

    def __init__(self, eng, fn):
        self.eng = eng
        self.fn = fn
        self.idx = -1
        self.waits = []
        self.signal = False
        self.count = 0
        self.vc = None
        self.dma = False
        self.dsem = -1
        self.dval = 0
        self.region = None


class Sched:
    def __init__(self):
        self.ops = {e: [] for e in ENGS}
        self.vc = {e: {} for e in ENGS}
        self.dma_last = [None] * N_DMA_SEMS
        self.dma_cnt = [0] * N_DMA_SEMS
        self.dma_rr2 = [0, 0]
        self.last_real = {e: None for e in ENGS}
        self.region = None
        self.region_pool = None
        self.skip_regs = {}

    @staticmethod
    def _tok(op):
        if op.dma:
            return (("d", op.dsem), op.dval)
        return (op.eng, op.idx)

    def _need(self, eng, op, prod, force=False):
        k, v = self._tok(prod)
        mine = self.vc[eng]
        if mine.get(k, -1) >= v and not force:
            return
        op.waits.append(prod)
        prod.signal = True
        for kk, vv in prod.vc.items():
            if mine.get(kk, -1) < vv:
                mine[kk] = vv

    def op(self, eng, fn, reads=(), writes=(), dma=False, after=()):
        op = Op(eng, fn)
        op.dma = dma
        if eng == "pool":
            op.region = self.region_pool
        elif self.region is not None and eng in ("pe", "act", "dve"):
            op.region = self.region
        lst = self.ops[eng]
        op.idx = len(lst)
        for b in reads:
            w = b.last_w
            if w is not None:
                if w.dma or w.eng != eng or eng in ("act", "dve", "pool"):
                    self._need(eng, op, w)
        strict = eng in ("act", "dve", "pool")
        for b in writes:
            w = b.last_w
            if w is not None and (w.dma or w.eng != eng or strict):
                self._need(eng, op, w)
            for r in b.readers:
                if r.dma or r.eng != eng or strict:
                    self._need(eng, op, r)
        for p in after:
            if p is not None and (p.dma or p.eng != eng):
                self._need(eng, op, p)
        if dma:
            half = N_DMA_SEMS // 2
            ri = 1 if eng == "pool" else 0
            k = ri * half + self.dma_rr2[ri]
            self.dma_rr2[ri] = (self.dma_rr2[ri] + 1) % half
            prev = self.dma_last[k]
            if prev is not None:
                self._need(eng, op, prev)
            self.dma_cnt[k] += 16
            op.dsem = k
            op.dval = self.dma_cnt[k]
            self.dma_last[k] = op
            op.signal = True
            vc = dict(self.vc[eng])
            vc[("d", k)] = op.dval
            op.vc = vc
        else:
            vc = dict(self.vc[eng])
            vc[eng] = op.idx
            op.vc = vc
            if fn is not None:
                self.last_real[eng] = op
        for b in reads:
            b.readers.append(op)
        for b in writes:
            b.last_w = op
            b.readers = []
        lst.append(op)
        return op

    def barrier(self, force=False):
        lasts = [self.last_real[e] for e in ENGS]
        dmas = [d for d in self.dma_last if d is not None]
        if not force:
            for e in ENGS:
                self.op(e, None, after=lasts + dmas)
            return
        for e in ENGS:
            self.vc[e] = {}
        for e in ENGS:
            op = self.op(e, None)
            for p in lasts + dmas:
                if p is not None and (p.dma or p.eng != e):
                    self._need(e, op, p, force=True)

    def emit(self, nc, stack):
        EPOCH = 4000
        esem = {}
        for e in ENGS:
            c = 0
            for op in self.ops[e]:
                if op.signal and not op.dma:
                    op.count = c % EPOCH + 1
                    op.dsem = c // EPOCH
                    c += 1
            nep = max(1, (c + EPOCH - 1) // EPOCH)
            esem[e] = [stack.enter_context(nc.semaphore("s_%s%d" % (e, i))) for i in range(nep)]
        assert max(self.dma_cnt) < 4096, max(self.dma_cnt)
        dsem = [stack.enter_context(nc.semaphore("d_%d" % i)) for i in range(N_DMA_SEMS)]
        block = stack.enter_context(nc.Block())

        def emit_op(eng, eng_name, op):
            for p in op.waits:
                if p.dma:
                    eng.wait_ge(dsem[p.dsem], p.dval)
                else:
                    eng.wait_ge(esem[p.eng][p.dsem], p.count)
            if op.fn is None:
                return
            ins = op.fn(eng)
            if op.dma:
                ins.then_inc(dsem[op.dsem], 16)
            elif op.signal:
                ins.then_inc(esem[eng_name][op.dsem], 1)

        def run(eng_name):
            def body(eng):
                ops = self.ops[eng_name]
                i = 0
                while i < len(ops):
                    op = ops[i]
                    if op.region is None:
                        emit_op(eng, eng_name, op)
                        i += 1
                        continue
                    j = i
                    while j < len(ops) and ops[j].region == op.region:
                        j += 1
                    grp = ops[i:j]
                    incs = {}
                    for o in grp:
                        if o.signal and not o.dma:
                            incs[o.dsem] = incs.get(o.dsem, 0) + 1
                    with eng.If_lt(self.skip_regs[eng_name], op.region + 1):
                        if eng_name == "pool":
                            for o in grp:
                                for p in o.waits:
                                    if p.dma:
                                        eng.wait_ge(dsem[p.dsem], p.dval)
                                    else:
                                        eng.wait_ge(esem[p.eng][p.dsem], p.count)
                                assert o.dma
                                eng.sem_inc(dsem[o.dsem], 16)
                        else:
                            if not incs:
                                eng.drain()
                            for ep, n in sorted(incs.items()):
                                eng.drain().then_inc(esem[eng_name][ep], n)
                    with eng.Else():
                        for o in grp:
                            emit_op(eng, eng_name, o)
                    i = j
            return body

        block.tensor(run("pe"))
        block.scalar(run("act"))
        block.vector(run("dve"))
        block.gpsimd(run("pool"))
        block.sync(run("sp"))


class T:
    __slots__ = ("ap", "b")

    def __init__(self, ap, name=""):
        self.ap = ap
        self.b = Buf(name)


class Arena:
    def __init__(self, ap):
        self.ap = ap
        self.off = 0
        self.n = ap.shape[1]
        self.top = self.n

    def mark(self):
        return self.off

    def release(self, m):
        self.off = m

    def tile(self, free, dtype, name="", top=False):
        if isinstance(free, int):
            free = (free,)
        nel = int(np.prod(free))
        sz = {F32: 4, I32: 4, BF16: 2}[dtype]
        cols = (nel * sz + 1) // 2
        cols = (cols + 15) // 16 * 16
        assert self.off + cols <= self.top, ("arena overflow", name, self.off, cols, self.top)
        if top:
            self.top -= cols
            v = self.ap[:, self.top:self.top + cols]
        else:
            v = self.ap[:, self.off:self.off + cols]
            self.off += cols
        if dtype != BF16:
            v = v.bitcast(dtype)
        v = v[:, 0:nel]
        if len(free) == 2:
            v = v.rearrange("p (a b) -> p a b", a=free[0])
        elif len(free) == 3:
            v = v.rearrange("p (a b c) -> p a b c", a=free[0], b=free[1])
        return T(v, name)


NCONST = 6 * 128 + 64 + 64 + 64 + 1


def make_consts():
    p = np.arange(128)[:, None]
    f = np.arange(128)[None, :]
    c = np.zeros((128, NCONST), np.float32)
    c[:, 0:128] = (p == f)
    c[:, 128:256] = (p < f)
    c[:, 256:384] = -(p >= f).astype(np.float32)
    c[:, 384:512] = -1.0
    c[:, 512:640] = (p // 64 == f // 64)
    c[:, 640:768] = (p <= f)
    e1 = np.arange(64)[None, :]
    c[:, 768:832] = (p < e1)
    c[:, 832:896] = (p <= e1)
    c[:, 896:960] = 512.0 * e1
    c[:, 960] = p[:, 0]
    return c


def build_program(debug=None):
    nc = bass.Bass("TRN2", target_bir_lowering=False)

    def din(name, shape, dt=F32):
        return nc.dram_tensor(name, list(shape), dt, kind="ExternalInput").ap()

    x = din("x", [S_TOK, D])
    consts = din("consts", [128, NCONST])
    w_in = din("w_in", [D, 2560])
    w_out = din("w_out", [D, D])
    n1w = din("n1w", [128, D])
    n2w = din("n2w", [128, D])
    sgunw = din("sgunw", [128, 512])
    onaw = din("onaw", [128, 512])
    qnw = din("qnw", [128, 64])
    knw = din("knw", [128, 64])
    sguwT = din("sguwT", [128, 4 * 128])
    sgubT = din("sgubT", [128, 4])
    onbw = din("onbw", [128, 4])
    rw = din("rw", [128, 8 * 32])
    rb = din("rb", [128, 32])
    need_moe = debug not in ("A", "B", "C")
    if need_moe:
        wgu = din("wgu", [NE * D, 2048])
        wd = din("wd", [NE * D, D])
        bgu = din("bgu", [NE * 128, 16])
        bd = din("bd", [NE, D])
    out = nc.dram_tensor("out", [S_TOK, D], F32, kind="ExternalOutput").ap()
    xs = nc.dram_tensor("xs_scr", [NSLOT, D], BF16, kind="Internal").ap()
    ys = nc.dram_tensor("ys_scr", [NSLOT, D], F32, kind="Internal").ap()

    S = Sched()
    st = ExitStack()
    with st:
        S.skip_regs = {"pe": st.enter_context(nc.tensor.register("nu_pe")),
                       "act": st.enter_context(nc.scalar.register("nu_act")),
                       "dve": st.enter_context(nc.vector.register("nu_dve")),
                       "pool": st.enter_context(nc.gpsimd.register("nu_pool"))}
        arena_ap = st.enter_context(nc.sbuf_tensor("arena", [128, 103000], BF16))
        A = Arena(arena_ap[:, :])
        PS = []
        PSW = []
        for i in range(4):
            pt = st.enter_context(nc.psum_tensor("psw%d" % i, [128, 1024], F32))
            PSW.append(pt[:, :].rearrange("p (c n) -> p c n", c=2))
            PS.append(T(pt[:, 0:512], "ps%d" % (2 * i)))
            PS.append(T(pt[:, 512:1024], "ps%d" % (2 * i + 1)))

        def psbf(i):
            return PS[i].ap.bitcast(BF16)

        cst = A.tile(NCONST, F32, "cst")
        S.op("sp", lambda e: e.dma_start(out=cst.ap, in_=consts[:, :]), writes=[cst.b], dma=True)
        identf = cst.ap[:, 0:128]
        cb = A.tile(6 * 128, BF16, "cb")
        S.op("dve", lambda e: e.tensor_copy(cb.ap, cst.ap[:, 0:768]), reads=[cst.b], writes=[cb.b])
        identb = cb.ap[:, 0:128]
        ltb = cb.ap[:, 128:256]
        negtri = cb.ap[:, 256:384]
        negones = cb.ap[:, 384:512]
        blkones = cb.ap[:, 512:640]
        onesb = A.tile(128, BF16, "onesb")
        S.op("dve", lambda e: e.memset(onesb.ap, 1.0), writes=[onesb.b])
        zeros = A.tile(512, BF16, "zeros")
        S.op("dve", lambda e: e.memset(zeros.ap, 0.0), writes=[zeros.b])

        def load_const(src, n, name, dt=F32):
            t = A.tile(n, dt, name)
            S.op("sp", lambda e: e.dma_start(out=t.ap, in_=src[:, :]), writes=[t.b], dma=True)
            return t

        n1w_t = load_const(n1w, D, "n1w")
        sgunw_t = load_const(sgunw, 512, "sgunw")
        onaw_t = load_const(onaw, 512, "onaw")
        qnw_t = load_const(qnw, 64, "qnw")
        knw_t = load_const(knw, 64, "knw")
        sgub_t = load_const(sgubT, 4, "sgub")
        onbw_t = load_const(onbw, 4, "onbw")
        rw_t = load_const(rw, 256, "rw")
        rb_t = load_const(rb, 32, "rb")
        S.op("dve", lambda e: e.tensor_scalar(qnw_t.ap, qnw_t.ap, 0.125, None, ALU.mult), reads=[qnw_t.b], writes=[qnw_t.b])
        sgw_f = load_const(sguwT, 512, "sgw_f")
        wmT = A.tile((4, 128), BF16, "wmT")
        S.op("dve", lambda e: e.tensor_tensor(wmT.ap, sgw_f.ap.rearrange("p (g t) -> p g t", g=4),
                                              cst.ap[:, 640:768].unsqueeze(1).to_broadcast([128, 4, 128]), ALU.mult),
             reads=[sgw_f.b, cst.b], writes=[wmT.b])

        destI = A.tile((NT, 4), I32, "destI", top=True)
        gsel = A.tile((NT, 4), F32, "gsel", top=True)
        widx = A.tile((NBLK, 8), I32, "widx", top=True)
        bidx = A.tile(NBLK, I32, "bidx", top=True)
        eidx = A.tile(NBLK, I32, "eidx", top=True)
        nuI = A.tile(1, I32, "nuI", top=True)
        top_persist = A.top
        mark0 = A.mark()
        qT = A.tile((4, S_TOK), BF16, "qT")
        kT = A.tile((4, S_TOK), BF16, "kT")
        Vt = A.tile((NT, 512), BF16, "V")
        markA = A.mark()

        zbig = A.tile(1024, BF16, "zbig")
        S.op("pool", lambda e: e.memset(zbig.ap, 0.0), writes=[zbig.b])
        xs_b = Buf("xs")
        xs_v = xs.rearrange("(n p) d -> n p d", p=128)
        xs_bufs = [Buf("xs%d" % n) for n in range(NSLOT // 128)]

        w_in_t = A.tile((8, 2560), BF16, "w_in")
        w_in_v = w_in.rearrange("(kc p) f -> p kc f", p=128)
        for kc in range(8):
            S.op("pool", (lambda kc: lambda e: e.dma_start(out=w_in_t.ap[:, kc, :], in_=w_in_v[:, kc, :]))(kc),
                 writes=[w_in_t.b], dma=True)
        w_out_t = A.tile((4, D), BF16, "w_out")
        w_out_v = w_out.rearrange("(kc p) f -> p kc f", p=128)
        for kc in range(4):
            S.op("pool", (lambda kc: lambda e: e.dma_start(out=w_out_t.ap[:, kc, :], in_=w_out_v[:, kc, :]))(kc),
                 writes=[w_out_t.b], dma=True)
        xt = [A.tile(D, F32, "xt%d" % i) for i in range(2)]
        sq = A.tile(D, BF16, "sq")
        st1 = A.tile(16, F32, "st1")
        hb = A.tile(D, BF16, "hb")
        hT = [A.tile((8, 128), BF16, "hT0")] * 2
        zt = A.tile(D, F32, "zt")
        vn = A.tile(512, BF16, "vn")
        ya = A.tile(512, F32, "ya")
        yan = A.tile(512, BF16, "yan")
        yaT = A.tile((4, 128), BF16, "yaT")
        qtmp = A.tile(512, F32, "qtmp")
        vtmp = qtmp
        qn = A.tile(512, BF16, "qn")
        kn = A.tile(512, BF16, "kn")
        x1p = A.tile(D, F32, "x1p")
        out_b = [Buf("out%d" % t) for t in range(NT)]

        epsT = A.tile(1, F32, "eps")
        S.op("dve", lambda e: e.memset(epsT.ap, EPS), writes=[epsT.b])

        def rstd_ops(ss_ap, n, ss_buf, dim):
            S.op("act", lambda e: e.activation(ss_ap, ss_ap, AF.Ln, bias=epsT.ap, scale=1.0 / dim), reads=[ss_buf, epsT.b], writes=[ss_buf])
            S.op("act", lambda e: e.activation(ss_ap, ss_ap, AF.Exp, scale=-0.5), reads=[ss_buf], writes=[ss_buf])

        def group_norm(src_ap, src_bufs, dst, groups, gdim, wt_ap, wt_buf, tmp, extra_scale_bcast_heads=False):
            n = groups * gdim
            S.op("act", lambda e: e.activation(sq.ap[:, 0:n], src_ap, AF.Square), reads=src_bufs, writes=[sq.b])
            ssv = st1.ap[:, 0:groups]
            S.op("dve", lambda e: e.tensor_reduce(ssv, sq.ap[:, 0:n].rearrange("p (g c) -> p g c", g=groups), AX.X, ALU.add),
                 reads=[sq.b], writes=[st1.b])
            rstd_ops(ssv, groups, st1.b, gdim)
            S.op("dve", lambda e: e.tensor_tensor(tmp.ap[:, 0:n].rearrange("p (g c) -> p g c", g=groups),
                                                  src_ap.rearrange("p (g c) -> p g c", g=groups),
                                                  ssv.unsqueeze(2).to_broadcast([128, groups, gdim]), ALU.mult),
                 reads=src_bufs + [st1.b], writes=[tmp.b])
            if extra_scale_bcast_heads:
                w_ap = wt_ap.unsqueeze(1).to_broadcast([128, groups, gdim])
                S.op("dve", lambda e: e.tensor_tensor(dst.ap.rearrange("p (g c) -> p g c", g=groups),
                                                      tmp.ap[:, 0:n].rearrange("p (g c) -> p g c", g=groups), w_ap, ALU.mult),
                     reads=[tmp.b, wt_buf], writes=[dst.b])
            else:
                S.op("dve", lambda e: e.tensor_tensor(dst.ap, tmp.ap[:, 0:n], wt_ap, ALU.mult), reads=[tmp.b, wt_buf], writes=[dst.b])

        for t in range(NT):
            xtt = xt[t % 2]
            hTt = hT[t % 2]
            if t == 0:
                S.op("sp", lambda e: e.dma_start(out=xt[0].ap, in_=x[0:128, :]), writes=[xt[0].b], dma=True)
            if t + 1 < NT:
                S.op("sp", (lambda t: lambda e: e.dma_start(out=xt[(t + 1) % 2].ap, in_=x[(t + 1) * 128:(t + 2) * 128, :]))(t),
                     writes=[xt[(t + 1) % 2].b], dma=True)
            for n in range(8 * t, 8 * t + 8):
                S.op("sp", (lambda n: lambda e: e.dma_start(out=xs_v[n], in_=zbig.ap))(n), reads=[zbig.b], writes=[xs_bufs[n]], dma=True)
            S.op("act", (lambda xtt: lambda e: e.activation(sq.ap, xtt.ap, AF.Square))(xtt), reads=[xtt.b], writes=[sq.b])
            S.op("dve", lambda e: e.tensor_reduce(st1.ap[:, 8:9], sq.ap, AX.X, ALU.add), reads=[sq.b], writes=[st1.b])
            rstd_ops(st1.ap[:, 8:9], 1, st1.b, D)
            S.op("dve", (lambda xtt: lambda e: e.scalar_tensor_tensor(hb.ap, xtt.ap, st1.ap[:, 8:9], n1w_t.ap, ALU.mult, ALU.mult))(xtt),
                 reads=[xtt.b, st1.b, n1w_t.b], writes=[hb.b])
            for kc in range(8):
                S.op("pe", (lambda kc: lambda e: e.transpose(psbf(0)[:, kc * 128:(kc + 1) * 128], hb.ap[:, kc * 128:(kc + 1) * 128], identb))(kc),
                     reads=[hb.b, cb.b], writes=[PS[0].b])
            S.op("act", (lambda hTt: lambda e: e.activation(hTt.ap, psbf(0).rearrange("p (a b) -> p a b", a=8), AF.Copy))(hTt),
                 reads=[PS[0].b], writes=[hTt.b])
            for cg in range(5):
                for kc in range(8):
                    S.op("pe", (lambda cg, kc, hTt: lambda e: e.matmul(PS[1 + cg].ap, hTt.ap[:, kc, :], w_in_t.ap[:, kc, cg * 512:(cg + 1) * 512],
                                                                    start=(kc == 0), stop=(kc == 7)))(cg, kc, hTt),
                         reads=[hTt.b, w_in_t.b], writes=[PS[1 + cg].b])
            S.op("act", (lambda t: lambda e: e.activation(Vt.ap[:, t, :], PS[5].ap, AF.Copy))(t), reads=[PS[5].b], writes=[Vt.b])
            S.op("act", lambda e: e.activation(zt.ap[:, 0:512], PS[1].ap, AF.Gelu_apprx_tanh), reads=[PS[1].b], writes=[zt.b])
            S.op("act", lambda e: e.activation(zt.ap[:, 512:1024], PS[2].ap, AF.Gelu_apprx_tanh), reads=[PS[2].b], writes=[zt.b])
            group_norm(zt.ap[:, 512:1024], [zt.b], vn, 4, 128, sgunw_t.ap, sgunw_t.b, vtmp)
            for g in range(4):
                S.op("pe", (lambda g: lambda e: e.matmul(PS[6].ap[:, g * 128:(g + 1) * 128], wmT.ap[:, g, :], vn.ap[:, g * 128:(g + 1) * 128],
                                                       start=True, stop=True))(g), reads=[wmT.b, vn.b], writes=[PS[6].b])
            S.op("dve", lambda e: e.tensor_tensor(ya.ap.rearrange("p (g c) -> p g c", g=4), PS[6].ap.rearrange("p (g c) -> p g c", g=4),
                                                  sgub_t.ap.unsqueeze(2).to_broadcast([128, 4, 128]), ALU.add),
                 reads=[PS[6].b, sgub_t.b], writes=[ya.b])
            S.op("dve", lambda e: e.tensor_tensor(ya.ap, ya.ap, zt.ap[:, 0:512], ALU.mult), reads=[ya.b, zt.b], writes=[ya.b])
            group_norm(ya.ap, [ya.b], yan, 4, 128, onaw_t.ap, onaw_t.b, vtmp)
            for c in range(4):
                S.op("pe", (lambda c: lambda e: e.transpose(psbf(7)[:, c * 128:(c + 1) * 128], yan.ap[:, c * 128:(c + 1) * 128], identb))(c),
                     reads=[yan.b, cb.b], writes=[PS[7].b])
            S.op("act", lambda e: e.activation(yaT.ap, psbf(7)[:, 0:512].rearrange("p (a b) -> p a b", a=4), AF.Copy),
                 reads=[PS[7].b], writes=[yaT.b])
            for half in range(2):
                for c in range(4):
                    S.op("pe", (lambda half, c: lambda e: e.matmul(PS[6].ap, yaT.ap[:, c, :], w_out_t.ap[:, c, half * 512:(half + 1) * 512],
                                                                 start=(c == 0), stop=(c == 3)))(half, c),
                         reads=[yaT.b, w_out_t.b], writes=[PS[6].b])
                S.op("dve", (lambda half, xtt: lambda e: e.tensor_tensor(x1p.ap[:, half * 512:(half + 1) * 512], PS[6].ap,
                                                                        xtt.ap[:, half * 512:(half + 1) * 512], ALU.add))(half, xtt),
                     reads=[PS[6].b, xtt.b], writes=[x1p.b])
            S.op("sp", (lambda t: lambda e: e.dma_start(out=out[t * 128:(t + 1) * 128, :], in_=x1p.ap))(t),
                 reads=[x1p.b], writes=[out_b[t]], dma=True)

            group_norm(PS[3].ap, [PS[3].b], qn, 8, 64, qnw_t.ap, qnw_t.b, qtmp, True)
            group_norm(PS[4].ap, [PS[4].b], kn, 8, 64, knw_t.ap, knw_t.b, qtmp, True)
            for c in range(4):
                S.op("pe", (lambda c: lambda e: e.transpose(psbf(7)[:, 512 + c * 128:512 + (c + 1) * 128], qn.ap[:, c * 128:(c + 1) * 128], identb))(c),
                     reads=[qn.b, cb.b], writes=[PS[7].b])
            S.op("act", (lambda t: lambda e: e.activation(qT.ap[:, :, t * 128:(t + 1) * 128], psbf(7)[:, 512:1024].rearrange("p (a b) -> p a b", a=4), AF.Copy))(t),
                 reads=[PS[7].b], writes=[qT.b])
            for c in range(4):
                S.op("pe", (lambda c: lambda e: e.transpose(psbf(0)[:, c * 128:(c + 1) * 128], kn.ap[:, c * 128:(c + 1) * 128], identb))(c),
                     reads=[kn.b, cb.b], writes=[PS[0].b])
            S.op("act", (lambda t: lambda e: e.activation(kT.ap[:, :, t * 128:(t + 1) * 128], psbf(0)[:, 0:512].rearrange("p (a b) -> p a b", a=4), AF.Copy))(t),
                 reads=[PS[0].b], writes=[kT.b])
        if debug == "A":
            dq = nc.dram_tensor("dbg_q", [128, 4 * S_TOK], BF16, kind="ExternalOutput").ap()
            dk = nc.dram_tensor("dbg_k", [128, 4 * S_TOK], BF16, kind="ExternalOutput").ap()
            dv = nc.dram_tensor("dbg_v", [128, NT * 512], BF16, kind="ExternalOutput").ap()
            S.op("sp", lambda e: e.dma_start(out=dq[:, :], in_=qT.ap.rearrange("p a b -> p (a b)")), reads=[qT.b], dma=True)
            S.op("sp", lambda e: e.dma_start(out=dk[:, :], in_=kT.ap.rearrange("p a b -> p (a b)")), reads=[kT.b], dma=True)
            S.op("sp", lambda e: e.dma_start(out=dv[:, :], in_=Vt.ap.rearrange("p a b -> p (a b)")), reads=[Vt.b], dma=True)
            S.barrier()
            S.emit(nc, st)
            return nc
        S.barrier()
        A.release(markA)
        _guard = A.tile(3072, BF16, "guard", top=True)
        yTb = A.tile((4, S_TOK), BF16, "yTb", top=True)
        qTm = A.tile((2, S_TOK), BF16, "qTm")
        Ew = A.tile((2, 512), F32, "Ew")
        SPw = [A.tile((2, 512), BF16, "SPw%d" % i) for i in range(2)]
        ATw = [A.tile((2, 512), BF16, "ATw%d" % i) for i in range(2)]
        Rw = [A.tile((2, 512), BF16, "Rw%d" % i) for i in range(2)]
        osq = A.tile(512, BF16, "osq")
        rsn = A.tile(512, F32, "rsn")
        PS_A, PS_B, PS_O, PS_SS = (PS[0], PS[1]), (PS[2], PS[3]), (PS[4], PS[5]), PS[6]
        PSA_w, PSB_w = PSW[0], PSW[1]
        ltb2 = ltb.unsqueeze(1).to_broadcast([128, 2, 128])
        S.op("pool", lambda e: e.memset(qTm.ap, 0.0), writes=[qTm.b])
        for j in range(4):
            S.op("dve", (lambda j: lambda e: e.tensor_copy(qTm.ap[0:64, 0, :], qT.ap[0:64, j, :]))(j), reads=[qT.b], writes=[qTm.b])
            S.op("pool", (lambda j: lambda e: e.tensor_copy(qTm.ap[64:128, 1, :], qT.ap[64:128, j, :]))(j), reads=[qT.b], writes=[qTm.b])

            def do_group(j, g):
                sbs = list(range(4 * g + 3, -1, -1))
                nst = len(sbs)

                def c0_of(s):
                    return max(0, sbs[s] - 4 * g) * 128

                for c in range(2):
                    S.op("pe", (lambda c: lambda e: e.matmul(PS_O[c].ap, Vt.ap[:, 0, 0:128], zeros.ap, start=True, stop=False))(c),
                         reads=[Vt.b, zeros.b], writes=[PS_O[c].b])
                S.op("pool", lambda e: e.memset(Rw[1].ap[:, :, 0:384], 0.0), writes=[Rw[1].b])
                S.op("pool", lambda e: e.memset(Rw[0].ap[:, :, 0:256], 0.0), writes=[Rw[0].b])

                def emitA(s, c):
                    sb, c0 = sbs[s], c0_of(s)
                    S.op("pe", lambda e: e.matmul(PS_A[c].ap[:, c0:512], kT.ap[:, j, sb * 128:(sb + 1) * 128],
                                                  qTm.ap[:, c, g * 512 + c0:(g + 1) * 512], start=True, stop=True),
                         reads=[kT.b, qTm.b], writes=[PS_A[c].b])

                def emit_front(s):
                    sb, c0 = sbs[s], c0_of(s)
                    sp_t = SPw[s % 2]
                    S.op("act", lambda e: e.activation(Ew.ap[:, :, c0:512], PSA_w[:, :, c0:512], AF.Exp),
                         reads=[PS_A[0].b, PS_A[1].b], writes=[Ew.b])
                    if s + 1 < nst:
                        for c in range(2):
                            emitA(s + 1, c)
                    S.op("act", lambda e: e.activation(sp_t.ap[:, :, c0:512], Ew.ap[:, :, c0:512], AF.Ln, bias=1.0), reads=[Ew.b], writes=[sp_t.b])
                    if sb >= 4 * g:
                        S.op("dve", lambda e: e.tensor_tensor(sp_t.ap[:, :, c0:c0 + 128], sp_t.ap[:, :, c0:c0 + 128], ltb2, ALU.mult),
                             reads=[sp_t.b, cb.b], writes=[sp_t.b])

                def emit_B(s):
                    sb, c0 = sbs[s], c0_of(s)
                    sp_t = SPw[s % 2]
                    for c in range(2):
                        S.op("pe", (lambda c: lambda e: e.matmul(PS_B[c].ap[:, c0:512], kT.ap[:, j, sb * 128:(sb + 1) * 128],
                                                               qTm.ap[:, c, g * 512 + c0:(g + 1) * 512], start=True, stop=False))(c),
                             reads=[kT.b, qTm.b], writes=[PS_B[c].b])
                        S.op("pe", (lambda c: lambda e: e.matmul(PS_B[c].ap[:, c0:512], negtri, sp_t.ap[:, c, c0:512], start=False, stop=(s == 0)))(c),
                             reads=[cb.b, sp_t.b], writes=[PS_B[c].b])
                        if s > 0:
                            S.op("pe", (lambda c: lambda e: e.matmul(PS_B[c].ap[:, c0:512], negones, Rw[s % 2].ap[:, c, c0:512], start=False, stop=True))(c),
                                 reads=[cb.b, Rw[s % 2].b], writes=[PS_B[c].b])
                    if s + 1 < nst:
                        r_n = Rw[(s + 1) % 2]
                        if s == 0:
                            S.op("pool", lambda e: e.tensor_copy(r_n.ap[:, :, c0:512], sp_t.ap[:, :, c0:512]), reads=[sp_t.b], writes=[r_n.b])
                        else:
                            r_c = Rw[s % 2]
                            S.op("dve", lambda e: e.tensor_tensor(r_n.ap[:, :, c0:512], r_c.ap[:, :, c0:512], sp_t.ap[:, :, c0:512], ALU.add),
                                 reads=[r_c.b, sp_t.b], writes=[r_n.b])

                def emit_back(s):
                    sb, c0 = sbs[s], c0_of(s)
                    at_t = ATw[s % 2]
                    S.op("act", lambda e: e.activation(at_t.ap[:, :, c0:512], PSB_w[:, :, c0:512], AF.Exp),
                         reads=[PS_B[0].b, PS_B[1].b], writes=[at_t.b])
                    if sb >= 4 * g:
                        S.op("dve", lambda e: e.tensor_tensor(at_t.ap[:, :, c0:c0 + 128], at_t.ap[:, :, c0:c0 + 128], ltb2, ALU.mult),
                             reads=[at_t.b, cb.b], writes=[at_t.b])
                    for c in range(2):
                        S.op("pe", (lambda c: lambda e: e.matmul(PS_O[c].ap[:, c0:512], Vt.ap[:, sb, j * 128:(j + 1) * 128], at_t.ap[:, c, c0:512],
                                                               start=False, stop=(s == nst - 1)))(c),
                             reads=[Vt.b, at_t.b], writes=[PS_O[c].b])

                for c in range(2):
                    emitA(0, c)
                for s in range(nst):
                    emit_front(s)
                    if s >= 1:
                        emit_back(s - 1)
                    emit_B(s)
                emit_back(nst - 1)
                for c in range(2):
                    S.op("act", (lambda c: lambda e: e.activation(osq.ap[c * 64:(c + 1) * 64, :], PS_O[c].ap[c * 64:(c + 1) * 64, :], AF.Square))(c),
                         reads=[PS_O[c].b], writes=[osq.b])
                S.op("pe", lambda e: e.matmul(PS_SS.ap, blkones, osq.ap, start=True, stop=True), reads=[cb.b, osq.b], writes=[PS_SS.b])
                S.op("act", lambda e: e.activation(rsn.ap, PS_SS.ap, AF.Ln, bias=epsT.ap, scale=1.0 / 64), reads=[PS_SS.b, epsT.b], writes=[rsn.b])
                S.op("act", lambda e: e.activation(rsn.ap, rsn.ap, AF.Exp, scale=-0.5), reads=[rsn.b], writes=[rsn.b])
                for c in range(2):
                    S.op("dve", (lambda c, j, g: lambda e: e.scalar_tensor_tensor(
                        yTb.ap[c * 64:(c + 1) * 64, j, g * 512:(g + 1) * 512], PS_O[c].ap[c * 64:(c + 1) * 64, :],
                        onbw_t.ap[c * 64:(c + 1) * 64, j:j + 1], rsn.ap[c * 64:(c + 1) * 64, :], ALU.mult, ALU.mult))(c, j, g),
                        reads=[PS_O[c].b, onbw_t.b, rsn.b], writes=[yTb.b])

            for g in range(8):
                do_group(j, g)

        if debug == "B":
            dy = nc.dram_tensor("dbg_y", [128, 4 * S_TOK], BF16, kind="ExternalOutput").ap()
            dk = nc.dram_tensor("dbg_k", [128, 4 * S_TOK], BF16, kind="ExternalOutput").ap()
            dv = nc.dram_tensor("dbg_v", [128, NT * 512], BF16, kind="ExternalOutput").ap()
            dq = nc.dram_tensor("dbg_q", [128, 2 * S_TOK], BF16, kind="ExternalOutput").ap()
            S.op("sp", lambda e: e.dma_start(out=dy[:, :], in_=yTb.ap.rearrange("p a b -> p (a b)")), reads=[yTb.b], dma=True)
            S.op("sp", lambda e: e.dma_start(out=dk[:, :], in_=kT.ap.rearrange("p a b -> p (a b)")), reads=[kT.b], dma=True)
            S.op("sp", lambda e: e.dma_start(out=dv[:, :], in_=Vt.ap.rearrange("p a b -> p (a b)")), reads=[Vt.b], dma=True)
            S.op("sp", lambda e: e.dma_start(out=dq[:, :], in_=qTm.ap.rearrange("p a b -> p (a b)")), reads=[qTm.b], dma=True)
            S.barrier()
            S.emit(nc, st)
            return nc

        S.barrier()
        A.release(mark0)
        h2s = A.tile((NT, D), BF16, "h2s")
        w_outB = A.tile((4, D), BF16, "w_outB")
        for kc in range(4):
            S.op("pool", (lambda kc: lambda e: e.dma_start(out=w_outB.ap[:, kc, :], in_=w_out_v[:, 4 + kc, :]))(kc), writes=[w_outB.b], dma=True)
        n2w_t = load_const(n2w, D, "n2w")
        maskS = A.tile((NT, 32), F32, "maskS")
        rankS = A.tile((NT, 32), F32, "rankS")
        gateS = A.tile((NT, 32), F32, "gateS")
        cumf = A.tile(32, F32, "cumf")
        cumb = A.tile(32, BF16, "cumb")
        maskb_l = [A.tile(32, BF16, "maskb%d" % i) for i in range(2)]
        x1t = [A.tile(D, F32, "x1t%d" % i) for i in range(2)]
        h2f_l = [A.tile(D, F32, "h2f%d" % i) for i in range(2)]
        sq2_l = [A.tile(D, F32, "sq2%d" % i) for i in range(2)]
        h2T_l = [A.tile((8, 128), F32, "h2T%d" % i) for i in range(2)]
        lg_l = [A.tile(32, F32, "lg%d" % i) for i in range(2)]
        ex_l = [A.tile(32, F32, "ex%d" % i) for i in range(2)]
        mx8_l = [A.tile(8, F32, "mx8%d" % i) for i in range(2)]
        sm_l = [A.tile(16, F32, "sm%d" % i) for i in range(2)]
        sq2, sm = sq2_l[0], sm_l[0]
        S.op("dve", lambda e: e.memset(cumf.ap, 0.0), writes=[cumf.b])

        def tileC1(t):
            xx = x1t[t % 2]
            pp = t % 2
            pb = 4 * pp
            h2f, sq2, h2T, lg, ex, mx8, sm, maskb = h2f_l[pp], sq2_l[pp], h2T_l[pp], lg_l[pp], ex_l[pp], mx8_l[pp], sm_l[pp], maskb_l[pp]
            S.op("sp", (lambda t, xx: lambda e: e.dma_start(out=xx.ap, in_=out[t * 128:(t + 1) * 128, :]))(t, xx),
                 reads=[out_b[t]], writes=[xx.b], dma=True)
            for half in range(2):
                for c in range(4):
                    S.op("pe", (lambda half, c, t: lambda e: e.matmul(PS[pb + half].ap, yTb.ap[:, c, t * 128:(t + 1) * 128],
                                                                    w_outB.ap[:, c, half * 512:(half + 1) * 512], start=(c == 0), stop=(c == 3)))(half, c, t),
                         reads=[yTb.b, w_outB.b], writes=[PS[pb + half].b])
                S.op("dve", (lambda half, xx: lambda e: e.tensor_tensor(xx.ap[:, half * 512:(half + 1) * 512], PS[pb + half].ap,
                                                                       xx.ap[:, half * 512:(half + 1) * 512], ALU.add))(half, xx),
                     reads=[PS[pb + half].b, xx.b], writes=[xx.b])
            S.op("sp", (lambda t, xx: lambda e: e.dma_start(out=out[t * 128:(t + 1) * 128, :], in_=xx.ap))(t, xx),
                 reads=[xx.b], writes=[out_b[t]], dma=True)
            S.op("act", (lambda xx: lambda e: e.activation(sq2.ap, xx.ap, AF.Square))(xx), reads=[xx.b], writes=[sq2.b])
            S.op("dve", lambda e: e.tensor_reduce(sm.ap[:, 0:1], sq2.ap, AX.X, ALU.add), reads=[sq2.b], writes=[sm.b])
            rstd_ops(sm.ap[:, 0:1], 1, sm.b, D)
            S.op("dve", (lambda xx: lambda e: e.scalar_tensor_tensor(h2f.ap, xx.ap, sm.ap[:, 0:1], n2w_t.ap, ALU.mult, ALU.mult))(xx),
                 reads=[xx.b, sm.b, n2w_t.b], writes=[h2f.b])
            S.op("pool", (lambda t: lambda e: e.tensor_copy(h2s.ap[:, t, :], h2f.ap))(t), reads=[h2f.b], writes=[h2s.b])
            for kc in range(8):
                S.op("pe", (lambda kc: lambda e: e.transpose(PS[pb + 2 + kc // 4].ap[:, (kc % 4) * 128:(kc % 4 + 1) * 128],
                                                              h2f.ap[:, kc * 128:(kc + 1) * 128], identf))(kc),
                     reads=[h2f.b, cst.b], writes=[PS[pb + 2 + kc // 4].b])
            for hh in range(2):
                S.op("act", (lambda hh: lambda e: e.activation(h2T.ap[:, hh * 4:(hh + 1) * 4, :], PS[pb + 2 + hh].ap.rearrange("p (a b) -> p a b", a=4), AF.Copy))(hh),
                     reads=[PS[pb + 2 + hh].b], writes=[h2T.b])
            for kc in range(8):
                S.op("pe", (lambda kc: lambda e: e.matmul(PS[pb].ap[:, 0:32], h2T.ap[:, kc, :], rw_t.ap[:, kc * 32:(kc + 1) * 32],
                                                        start=(kc == 0), stop=(kc == 7)))(kc),
                     reads=[h2T.b, rw_t.b], writes=[PS[pb].b])

        def tileC2(t):
            xx = x1t[t % 2]
            pp = t % 2
            pb = 4 * pp
            h2f, sq2, h2T, lg, ex, mx8, sm, maskb = h2f_l[pp], sq2_l[pp], h2T_l[pp], lg_l[pp], ex_l[pp], mx8_l[pp], sm_l[pp], maskb_l[pp]
            S.op("dve", lambda e: e.tensor_tensor(lg.ap, PS[pb].ap[:, 0:32], rb_t.ap, ALU.add), reads=[PS[pb].b, rb_t.b], writes=[lg.b])
            S.op("dve", lambda e: e.max(mx8.ap, lg.ap), reads=[lg.b], writes=[mx8.b])
            S.op("dve", (lambda t: lambda e: e.tensor_scalar(maskS.ap[:, t, :], lg.ap, mx8.ap[:, 3:4], None, ALU.is_ge))(t),
                 reads=[lg.b, mx8.b], writes=[maskS.b])
            S.op("dve", lambda e: e.tensor_scalar(sm.ap[:, 1:2], mx8.ap[:, 0:1], -1.0, None, ALU.mult), reads=[mx8.b], writes=[sm.b])
            S.op("act", lambda e: e.activation(ex.ap, lg.ap, AF.Exp, bias=sm.ap[:, 1:2]), reads=[lg.b, sm.b], writes=[ex.b])
            S.op("dve", (lambda t: lambda e: e.tensor_tensor(ex.ap, ex.ap, maskS.ap[:, t, :], ALU.mult))(t), reads=[ex.b, maskS.b], writes=[ex.b])
            S.op("dve", lambda e: e.tensor_reduce(sm.ap[:, 2:3], ex.ap, AX.X, ALU.add), reads=[ex.b], writes=[sm.b])
            S.op("dve", lambda e: e.reciprocal(sm.ap[:, 2:3], sm.ap[:, 2:3]), reads=[sm.b], writes=[sm.b])
            S.op("dve", (lambda t: lambda e: e.tensor_scalar(gateS.ap[:, t, :], ex.ap, sm.ap[:, 2:3], None, ALU.mult))(t),
                 reads=[ex.b, sm.b], writes=[gateS.b])
            S.op("dve", (lambda t: lambda e: e.tensor_copy(maskb.ap, maskS.ap[:, t, :]))(t), reads=[maskS.b], writes=[maskb.b])
            S.op("pe", lambda e: e.matmul(PS[pb + 1].ap[:, 0:32], ltb, maskb.ap, start=True, stop=(t == 0)), reads=[cb.b, maskb.b], writes=[PS[pb + 1].b])
            if t > 0:
                S.op("pe", lambda e: e.matmul(PS[pb + 1].ap[:, 0:32], onesb.ap, cumb.ap, start=False, stop=True), reads=[onesb.b, cumb.b], writes=[PS[pb + 1].b])
            S.op("act", (lambda t: lambda e: e.activation(rankS.ap[:, t, :], PS[pb + 1].ap[:, 0:32], AF.Copy))(t), reads=[PS[pb + 1].b], writes=[rankS.b])
            S.op("dve", (lambda t: lambda e: e.tensor_tensor(cumf.ap, cumf.ap, maskS.ap[:, t, :], ALU.add))(t), reads=[cumf.b, maskS.b], writes=[cumf.b])
            S.op("dve", lambda e: e.tensor_copy(cumb.ap, cumf.ap), reads=[cumf.b], writes=[cumb.b])
        tileC1(0)
        for t in range(NT):
            if t + 1 < NT:
                tileC1(t + 1)
            tileC2(t)
        cnt = A.tile(128, F32, "cnt")
        pst = A.tile(64, F32, "pst")
        big = A.tile((NBLK, 32), F32, "big")
        ebf = A.tile(NBLK, F32, "ebf")
        wf = A.tile((NBLK, 8), F32, "wf")
        S.op("pe", lambda e: e.matmul(PS[6].ap[0:32, 0:128], cumb.ap, onesb.ap, start=True, stop=True), reads=[cumb.b, onesb.b], writes=[PS[6].b])
        S.op("dve", lambda e: e.tensor_copy(cnt.ap[0:32, :], PS[6].ap[0:32, 0:128]), reads=[PS[6].b], writes=[cnt.b])
        S.op("dve", lambda e: e.tensor_tensor(sq2.ap[0:32, 0:64], cnt.ap[0:32, 0:64], cst.ap[0:32, 896:960], ALU.is_gt), reads=[cnt.b, cst.b], writes=[sq2.b])
        S.op("dve", lambda e: e.tensor_reduce(sm.ap[0:32, 4:5], sq2.ap[0:32, 0:64], AX.X, ALU.add), reads=[sq2.b], writes=[sm.b])
        S.op("dve", lambda e: e.tensor_scalar(cnt.ap[0:32, :], cst.ap[0:32, 384:512], sm.ap[0:32, 4:5], -512.0, ALU.mult, ALU.mult), reads=[cst.b, sm.b], writes=[cnt.b])
        S.op("pe", lambda e: e.matmul(PS[7].ap[:, 0:32], cnt.ap[0:32, :], cst.ap[0:32, 768:800], start=True, stop=True), reads=[cnt.b, cst.b], writes=[PS[7].b])
        S.op("pe", lambda e: e.matmul(PS[7].ap[:, 32:64], cnt.ap[0:32, :], cst.ap[0:32, 832:864], start=True, stop=True), reads=[cnt.b, cst.b], writes=[PS[7].b])
        S.op("dve", lambda e: e.tensor_copy(pst.ap, PS[7].ap[:, 0:64]), reads=[PS[7].b], writes=[pst.b])
        S.op("dve", lambda e: e.tensor_tensor(big.ap, pst.ap[:, 32:64].unsqueeze(1).to_broadcast([128, NBLK, 32]),
                                              cst.ap[:, 896:960].unsqueeze(2).to_broadcast([128, NBLK, 32]), ALU.is_le),
             reads=[pst.b, cst.b], writes=[big.b])
        S.op("dve", lambda e: e.tensor_reduce(ebf.ap, big.ap, AX.X, ALU.add), reads=[big.b], writes=[ebf.b])
        S.op("dve", lambda e: e.tensor_scalar(ebf.ap, ebf.ap, 31.0, None, ALU.min), reads=[ebf.b], writes=[ebf.b])
        nuf = A.tile(1, F32, "nuf")
        S.op("dve", lambda e: e.tensor_scalar(nuf.ap, pst.ap[:, 63:64], 1.0 / 512.0, None, ALU.mult), reads=[pst.b], writes=[nuf.b])
        S.op("dve", lambda e: e.tensor_copy(nuI.ap, nuf.ap), reads=[nuf.b], writes=[nuI.b])
        S.op("dve", lambda e: e.tensor_copy(eidx.ap, ebf.ap), reads=[ebf.b], writes=[eidx.b])
        S.op("dve", lambda e: e.tensor_scalar(sq2.ap[:, 0:NBLK], ebf.ap, 128.0, cst.ap[:, 960:961], ALU.mult, ALU.add), reads=[ebf.b, cst.b], writes=[sq2.b])
        S.op("dve", lambda e: e.tensor_copy(bidx.ap, sq2.ap[:, 0:NBLK]), reads=[sq2.b], writes=[bidx.b])
        S.op("dve", lambda e: e.tensor_scalar(sq2.ap[:, 0:NBLK], ebf.ap, 1024.0, cst.ap[:, 960:961], ALU.mult, ALU.add), reads=[ebf.b, cst.b], writes=[sq2.b])
        for kc in range(8):
            S.op("dve", (lambda kc: lambda e: e.tensor_scalar(wf.ap[:, :, kc], sq2.ap[:, 0:NBLK], float(kc * 128), None, ALU.add))(kc),
                 reads=[sq2.b], writes=[wf.b])
        S.op("dve", lambda e: e.tensor_copy(widx.ap, wf.ap), reads=[wf.b], writes=[widx.b])
        S.op("dve", lambda e: e.tensor_scalar(pst.ap[:, 0:32], pst.ap[:, 0:32], 1.0, None, ALU.add), reads=[pst.b], writes=[pst.b])
        val = A.tile(32, F32, "val")
        eqt = A.tile(32, F32, "eqt")
        top8 = A.tile(8, F32, "top8")
        dsf = A.tile(4, F32, "dsf")
        scat_ops = []
        for t in range(NT):
            S.op("dve", (lambda t: lambda e: e.tensor_tensor(val.ap, rankS.ap[:, t, :], pst.ap[:, 0:32], ALU.add))(t), reads=[rankS.b, pst.b], writes=[val.b])
            S.op("dve", (lambda t: lambda e: e.tensor_tensor(val.ap, val.ap, maskS.ap[:, t, :], ALU.mult))(t), reads=[val.b, maskS.b], writes=[val.b])
            S.op("dve", lambda e: e.max(top8.ap, val.ap), reads=[val.b], writes=[top8.b])
            for k in range(4):
                S.op("dve", (lambda k: lambda e: e.tensor_scalar(eqt.ap, val.ap, top8.ap[:, k:k + 1], None, ALU.is_equal))(k), reads=[val.b, top8.b], writes=[eqt.b])
                S.op("dve", (lambda t: lambda e: e.tensor_tensor(eqt.ap, eqt.ap, gateS.ap[:, t, :], ALU.mult))(t), reads=[eqt.b, gateS.b], writes=[eqt.b])
                S.op("dve", (lambda t, k: lambda e: e.tensor_reduce(gsel.ap[:, t, k:k + 1], eqt.ap, AX.X, ALU.add))(t, k), reads=[eqt.b], writes=[gsel.b])
            S.op("dve", lambda e: e.tensor_scalar(dsf.ap, top8.ap[:, 0:4], -1.0, None, ALU.add), reads=[top8.b], writes=[dsf.b])
            S.op("dve", (lambda t: lambda e: e.tensor_copy(destI.ap[:, t, :], dsf.ap))(t), reads=[dsf.b], writes=[destI.b])
            for k in range(4):
                scat_ops.append(S.op("pool", (lambda t, k: lambda e: e.indirect_dma_start(
                    out=xs[:, :], out_offset=bass.IndirectOffsetOnAxis(ap=destI.ap[:, t, k:k + 1], axis=0),
                    in_=h2s.ap[:, t, :], in_offset=None))(t, k), reads=[h2s.b, destI.b], dma=True))

        if debug == "C":
            dd = nc.dram_tensor("dbg_dest", [128, NT * 4], I32, kind="ExternalOutput").ap()
            dg = nc.dram_tensor("dbg_gsel", [128, NT * 4], F32, kind="ExternalOutput").ap()
            de = nc.dram_tensor("dbg_eidx", [128, NBLK], I32, kind="ExternalOutput").ap()
            dw = nc.dram_tensor("dbg_widx", [128, NBLK * 8], I32, kind="ExternalOutput").ap()
            dxs = nc.dram_tensor("dbg_xs", [2048, D], BF16, kind="ExternalOutput").ap()
            S.op("sp", lambda e: e.dma_start(out=dd[:, :], in_=destI.ap.rearrange("p a b -> p (a b)")), reads=[destI.b], dma=True)
            S.op("sp", lambda e: e.dma_start(out=dg[:, :], in_=gsel.ap.rearrange("p a b -> p (a b)")), reads=[gsel.b], dma=True)
            S.op("sp", lambda e: e.dma_start(out=de[:, :], in_=eidx.ap), reads=[eidx.b], dma=True)
            S.op("sp", lambda e: e.dma_start(out=dw[:, :], in_=widx.ap.rearrange("p a b -> p (a b)")), reads=[widx.b], dma=True)
            S.barrier()
            S.op("sp", lambda e: e.dma_start(out=dxs[:, :], in_=xs[0:2048, :]), dma=True)
            S.barrier()
            S.emit(nc, st)
            return nc

        S.barrier()
        A.release(mark0)
        A.top = top_persist
        Wg = [A.tile((8, 2048), BF16, "Wg%d" % i) for i in range(2)]
        Wd = [A.tile((8, D), BF16, "Wd%d" % i) for i in range(2)]
        Wg_b = [[Buf() for _ in range(8)] for _ in range(2)]
        Wd_b = [[Buf() for _ in range(8)] for _ in range(2)]
        xin = [A.tile((4, D), BF16, "xin%d" % i) for i in range(2)]
        xTt = A.tile((8, 512), BF16, "xT")
        actT = A.tile((8, 512), BF16, "actT")
        bgu_t = [A.tile(16, F32, "bgu%d" % i) for i in range(2)]
        bd_t = [A.tile(D, F32, "bd%d" % i) for i in range(2)]
        g1 = [A.tile(512, F32, "g1%d" % i) for i in range(2)]
        sg = [A.tile(512, F32, "sg%d" % i) for i in range(2)]
        u1 = [A.tile(512, F32, "u1%d" % i) for i in range(2)]
        yo = [A.tile(D, F32, "yo%d" % i) for i in range(2)]
        ys_b = Buf("ys")
        PS_G, PS_U, PS_Y = (PS[1], PS[2]), (PS[3], PS[4]), (PS[5], PS[6])

        def load_block(b):
            bf = b % 2
            S.region_pool = b if b >= 32 else None
            S.op("sp", lambda e: e.dma_start(out=xin[bf].ap, in_=xs[b * BLK:(b + 1) * BLK, :].rearrange("(j p) d -> p j d", p=128)),
                 writes=[xin[bf].b], dma=True)
            S.op("pool", lambda e: e.indirect_dma_start(out=bgu_t[bf].ap, out_offset=None, in_=bgu[:, :],
                                                        in_offset=bass.IndirectOffsetOnAxis(ap=bidx.ap[:, b:b + 1], axis=0)),
                 reads=[bidx.b], writes=[bgu_t[bf].b], dma=True)
            S.op("pool", lambda e: e.indirect_dma_start(out=bd_t[bf].ap, out_offset=None, in_=bd[:, :],
                                                        in_offset=bass.IndirectOffsetOnAxis(ap=eidx.ap[:, b:b + 1], axis=0)),
                 reads=[eidx.b], writes=[bd_t[bf].b], dma=True)
            for kc in range(8):
                S.op("pool", (lambda kc: lambda e: e.indirect_dma_start(out=Wg[bf].ap[:, kc, :], out_offset=None, in_=wgu[:, :],
                                                                        in_offset=bass.IndirectOffsetOnAxis(ap=widx.ap[:, b, kc:kc + 1], axis=0)))(kc),
                     reads=[widx.b], writes=[Wg_b[bf][kc]], dma=True)
            for kc in range(8):
                S.op("pool", (lambda kc: lambda e: e.indirect_dma_start(out=Wd[bf].ap[:, kc, :], out_offset=None, in_=wd[:, :],
                                                                        in_offset=bass.IndirectOffsetOnAxis(ap=widx.ap[:, b, kc:kc + 1], axis=0)))(kc),
                     reads=[widx.b], writes=[Wd_b[bf][kc]], dma=True)

        for en in ("pe", "act", "dve", "pool"):
            S.op(en, (lambda en: lambda e: e.reg_load(S.skip_regs[en], nuI.ap[0:1, 0:1]))(en), reads=[nuI.b])
        load_block(0)

        def do_block(b):
            bf = b % 2
            S.region = b if b >= 32 else None
            if b + 1 < NBLK:
                load_block(b + 1)
            for kp in range(4):
                for kk in range(2):
                    kc = kp * 2 + kk
                    for jj in range(4):
                        S.op("pe", (lambda kc, kk, jj: lambda e: e.transpose(psbf(0)[:, kk * 512 + jj * 128: kk * 512 + (jj + 1) * 128],
                                                                              xin[bf].ap[:, jj, kc * 128:(kc + 1) * 128], identb))(kc, kk, jj),
                             reads=[xin[bf].b, cb.b], writes=[PS[0].b])
                S.op("act", (lambda kp: lambda e: e.activation(xTt.ap[:, kp * 2:kp * 2 + 2, :], psbf(0).rearrange("p (a b) -> p a b", a=2), AF.Copy))(kp),
                     reads=[PS[0].b], writes=[xTt.b])
            for m in range(8):
                pg, pu = PS_G[m % 2], PS_U[m % 2]
                g1t, sgt, u1t = g1[m % 2], sg[m % 2], u1[m % 2]
                for kc in range(8):
                    S.op("pe", (lambda m, kc, pg: lambda e: e.matmul(pg.ap, Wg[bf].ap[:, kc, m * 128:(m + 1) * 128], xTt.ap[:, kc, :],
                                                                   start=(kc == 0), stop=(kc == 7)))(m, kc, pg),
                         reads=[Wg_b[bf][kc], xTt.b], writes=[pg.b])
                for kc in range(8):
                    S.op("pe", (lambda m, kc, pu: lambda e: e.matmul(pu.ap, Wg[bf].ap[:, kc, 1024 + m * 128:1024 + (m + 1) * 128], xTt.ap[:, kc, :],
                                                                   start=(kc == 0), stop=(kc == 7)))(m, kc, pu),
                         reads=[Wg_b[bf][kc], xTt.b], writes=[pu.b])
                S.op("dve", (lambda m, pg, g1t: lambda e: e.tensor_scalar(g1t.ap, pg.ap, bgu_t[bf].ap[:, m:m + 1], 7.0, ALU.add, ALU.min))(m, pg, g1t),
                     reads=[pg.b, bgu_t[bf].b], writes=[g1t.b])
                S.op("act", (lambda g1t, sgt: lambda e: e.activation(sgt.ap, g1t.ap, AF.Sigmoid, scale=1.702))(g1t, sgt), reads=[g1t.b], writes=[sgt.b])
                S.op("dve", (lambda m, pu, u1t: lambda e: e.tensor_scalar(u1t.ap, pu.ap, bgu_t[bf].ap[:, 8 + m:9 + m], 7.0, ALU.add, ALU.min))(m, pu, u1t),
                     reads=[pu.b, bgu_t[bf].b], writes=[u1t.b])
                S.op("dve", (lambda u1t: lambda e: e.tensor_scalar(u1t.ap, u1t.ap, -7.0, 1.0, ALU.max, ALU.add))(u1t), reads=[u1t.b], writes=[u1t.b])
                S.op("dve", (lambda g1t, sgt: lambda e: e.tensor_tensor(g1t.ap, g1t.ap, sgt.ap, ALU.mult))(g1t, sgt), reads=[g1t.b, sgt.b], writes=[g1t.b])
                S.op("dve", (lambda m, g1t, u1t: lambda e: e.tensor_tensor(actT.ap[:, m, :], u1t.ap, g1t.ap, ALU.mult))(m, g1t, u1t),
                     reads=[u1t.b, g1t.b], writes=[actT.b])
            for jj in range(4):
                yot = yo[jj % 2]
                for half in range(2):
                    py = PS_Y[half]
                    for m in range(8):
                        S.op("pe", (lambda jj, half, m, py: lambda e: e.matmul(py.ap, actT.ap[:, m, jj * 128:(jj + 1) * 128],
                                                                             Wd[bf].ap[:, m, half * 512:(half + 1) * 512],
                                                                             start=(m == 0), stop=(m == 7)))(jj, half, m, py),
                             reads=[actT.b, Wd_b[bf][m]], writes=[py.b])
                    S.op("dve", (lambda half, py, yot: lambda e: e.tensor_tensor(yot.ap[:, half * 512:(half + 1) * 512], py.ap,
                                                                                bd_t[bf].ap[:, half * 512:(half + 1) * 512], ALU.add))(half, py, yot),
                         reads=[py.b, bd_t[bf].b], writes=[yot.b])
                S.op("sp", (lambda jj, yot: lambda e: e.dma_start(out=ys[b * BLK + jj * 128:b * BLK + (jj + 1) * 128, :], in_=yot.ap))(jj, yot),
                     reads=[yot.b], dma=True)

        for b in range(NBLK):
            do_block(b)
            S.region = None
        S.region_pool = None

        S.barrier(force=True)
        A.release(mark0)
        Yg = [A.tile((4, D), F32, "Yg%d" % i) for i in range(4)]
        xo = [A.tile(D, F32, "xo%d" % i) for i in range(4)]
        for t in range(NT):
            yg, xx = Yg[t % 4], xo[t % 4]
            S.op("sp", (lambda t, xx: lambda e: e.dma_start(out=xx.ap, in_=out[t * 128:(t + 1) * 128, :]))(t, xx),
                 reads=[out_b[t]], writes=[xx.b], dma=True)
            for k in range(4):
                S.op("pool", (lambda t, k, yg: lambda e: e.indirect_dma_start(out=yg.ap[:, k, :], out_offset=None, in_=ys[:, :],
                                                                              in_offset=bass.IndirectOffsetOnAxis(ap=destI.ap[:, t, k:k + 1], axis=0)))(t, k, yg),
                     reads=[destI.b], writes=[yg.b], dma=True)
            for k in range(4):
                S.op("dve", (lambda t, k, yg, xx: lambda e: e.scalar_tensor_tensor(xx.ap, yg.ap[:, k, :], gsel.ap[:, t, k:k + 1], xx.ap, ALU.mult, ALU.add))(t, k, yg, xx),
                     reads=[yg.b, gsel.b, xx.b], writes=[xx.b])
            S.op("sp", (lambda t, xx: lambda e: e.dma_start(out=out[t * 128:(t + 1) * 128, :], in_=xx.ap))(t, xx),
                 reads=[xx.b], writes=[out_b[t]], dma=True)
        S.barrier()
        S.emit(nc, st)
    return nc


def _rep(v, n=128):
    return np.ascontiguousarray(np.broadcast_to(np.asarray(v, np.float32).reshape(1, -1), (n, v.size)))


def prep_shared(inp):
    f = lambda a: np.ascontiguousarray(np.asarray(a, np.float32))
    sh = {}
    sh["consts"] = make_consts()
    sh["w_in"] = f(inp["w_in"][0])
    sh["w_out"] = f(inp["w_out"][0])
    sh["n1w"] = _rep(inp["norm1_w"][0])
    sh["n2w"] = _rep(inp["norm2_w"][0])
    sh["sgunw"] = _rep(inp["sgu_norm_w"][0])
    sh["onaw"] = _rep(inp["out_norm_a_w"][0])
    sh["qnw"] = _rep(inp["q_norm_w"][0])
    sh["knw"] = _rep(inp["k_norm_w"][0])
    sh["sguwT"] = f(np.asarray(inp["sgu_w"][0]).transpose(2, 0, 1).reshape(128, 512))
    sh["sgubT"] = f(np.asarray(inp["sgu_b"][0]).T)
    sh["onbw"] = f(np.asarray(inp["out_norm_b_w"][0]).reshape(4, 128).T)
    sh["rw"] = f(np.asarray(inp["router_w"][0]).reshape(8, 128, 32).transpose(1, 0, 2).reshape(128, 256))
    sh["rb"] = _rep(inp["router_b"][0])
    wg = np.asarray(inp["w_gate_up"][0], np.float32).reshape(NE, D, D, 2)
    sh["wgu"] = np.ascontiguousarray(wg.transpose(0, 1, 3, 2).reshape(NE * D, 2048))
    sh["wd"] = f(np.asarray(inp["w_down"][0]).reshape(NE * D, D))
    if os.environ.get("KDEBUG") in ("A", "B", "C"):
        del sh["wgu"], sh["wd"]
        return sh
    b = np.asarray(inp["b_gate_up"][0], np.float32).reshape(NE, D, 2)
    bg = b[:, :, 0].reshape(NE, 8, 128).transpose(0, 2, 1)
    bu = b[:, :, 1].reshape(NE, 8, 128).transpose(0, 2, 1)
    sh["bgu"] = np.ascontiguousarray(np.concatenate([bg, bu], axis=2).reshape(NE * 128, 16))
    sh["bd"] = f(inp["b_down"][0])
    return sh


def kernel(**inputs):
    debug = os.environ.get("KDEBUG") or None
    nc = build_program(debug)
    sh = prep_shared(inputs)
    xfull = np.asarray(inputs["x"], np.float32)
    ncores = 8 if debug is None else int(os.environ.get("KCORES", "1"))
    in_maps = []
    for c in range(ncores):
        m = dict(sh)
        m["x"] = np.ascontiguousarray(xfull[c])
        in_maps.append(m)
    res = run_bass_kernel_spmd(nc, in_maps, core_ids=list(range(ncores)))
    outs = [np.asarray(r["out"]) for r in res.results]
    if debug is not None:
        return res.results
    return np.stack(outs, axis=0).astype(np.float32)
```

```python
import os
import numpy as np
from contextlib import ExitStack
import concourse.bass as bass
import concourse.mybir as mybir
from concourse.bass_utils import run_bass_kernel_spmd

F32 = mybir.dt.float32
BF16 = mybir.dt.bfloat16
I32 = mybir.dt.int32
AF = mybir.ActivationFunctionType
ALU = mybir.AluOpType
AX = mybir.AxisListType

S_TOK = 4096
D = 1024
NT = S_TOK // 128
NE = 32
NBLK = 64
BLK = 512
NSLOT = NBLK * BLK
EPS = 1e-5

ENGS = ("pe", "act", "dve", "pool", "sp")
N_DMA_SEMS = 48


class Buf:
    __slots__ = ("name", "last_w", "readers")

    def __init__(self, name=""):
        self.name = name
        self.last_w = None
        self.readers = []


class Op:
    __slots__ = ("eng", "fn", "idx", "waits", "signal", "count", "vc", "dma", "dsem", "dval", "region")

    def __init__(self, eng, fn):
        self.eng = eng
        self.fn = fn
        self.idx = -1
        self.waits = []
        self.signal = False
        self.count = 0
        self.vc = None
        self.dma = False
        self.dsem = -1
        self.dval = 0
        self.region = None


class Sched:
    def __init__(self):
        self.ops = {e: [] for e in ENGS}
        self.vc = {e: {} for e in ENGS}
        self.dma_last = [None] * N_DMA_SEMS
        self.dma_cnt = [0] * N_DMA_SEMS
        self.dma_rr2 = [0, 0]
        self.last_real = {e: None for e in ENGS}
        self.region = None
        self.region_pool = None
        self.skip_regs = {}

    @staticmethod
    def _tok(op):
        if op.dma:
            return (("d", op.dsem), op.dval)
        return (op.eng, op.idx)

    def _need(self, eng, op, prod, force=False):
        k, v = self._tok(prod)
        mine = self.vc[eng]
        if mine.get(k, -1) >= v and not force:
            return
        op.waits.append(prod)
        prod.signal = True
        for kk, vv in prod.vc.items():
            if mine.get(kk, -1) < vv:
                mine[kk] = vv

    def op(self, eng, fn, reads=(), writes=(), dma=False, after=()):
        op = Op(eng, fn)
        op.dma = dma
        if eng == "pool":
            op.region = self.region_pool
        elif self.region is not None and eng in ("pe", "act", "dve"):
            op.region = self.region
        lst = self.ops[eng]
        op.idx = len(lst)
        for b in reads:
            w = b.last_w
            if w is not None:
                if w.dma or w.eng != eng or eng in ("act", "dve", "pool"):
                    self._need(eng, op, w)
        strict = eng in ("act", "dve", "pool")
        for b in writes:
            w = b.last_w
            if w is not None and (w.dma or w.eng != eng or strict):
                self._need(eng, op, w)
            for r in b.readers:
                if r.dma or r.eng != eng or strict:
                    self._need(eng, op, r)
        for p in after:
            if p is not None and (p.dma or p.eng != eng):
                self._need(eng, op, p)
        if dma:
            half = N_DMA_SEMS // 2
            ri = 1 if eng == "pool" else 0
            k = ri * half + self.dma_rr2[ri]
            self.dma_rr2[ri] = (self.dma_rr2[ri] + 1) % half
            prev = self.dma_last[k]
            if prev is not None:
                self._need(eng, op, prev)
            self.dma_cnt[k] += 16
            op.dsem = k
            op.dval = self.dma_cnt[k]
            self.dma_last[k] = op
            op.signal = True
            vc = dict(self.vc[eng])
            vc[("d", k)] = op.dval
            op.vc = vc
        else:
            vc = dict(self.vc[eng])
            vc[eng] = op.idx
            op.vc = vc
            if fn is not None:
                self.last_real[eng] = op
        for b in reads:
            b.readers.append(op)
        for b in writes:
            b.last_w = op
            b.readers = []
        lst.append(op)
        return op

    def barrier(self, force=False):
        lasts = [self.last_real[e] for e in ENGS]
        dmas = [d for d in self.dma_last if d is not None]
        if not force:
            for e in ENGS:
                self.op(e, None, after=lasts + dmas)
            return
        for e in ENGS:
            self.vc[e] = {}
        for e in ENGS:
            op = self.op(e, None)
            for p in lasts + dmas:
                if p is not None and (p.dma or p.eng != e):
                    self._need(e, op, p, force=True)

    def emit(self, nc, stack):
        EPOCH = 4000
        esem = {}
        for e in ENGS:
            c = 0
            for op in self.ops[e]:
                if op.signal and not op.dma:
                    op.count = c % EPOCH + 1
                    op.dsem = c // EPOCH
                    c += 1
            nep = max(1, (c + EPOCH - 1) // EPOCH)
            esem[e] = [stack.enter_context(nc.semaphore("s_%s%d" % (e, i))) for i in range(nep)]
        assert max(self.dma_cnt) < 4096, max(self.dma_cnt)
        dsem = [stack.enter_context(nc.semaphore("d_%d" % i)) for i in range(N_DMA_SEMS)]
        block = stack.enter_context(nc.Block())

        def emit_op(eng, eng_name, op):
            for p in op.waits:
                if p.dma:
                    eng.wait_ge(dsem[p.dsem], p.dval)
                else:
                    eng.wait_ge(esem[p.eng][p.dsem], p.count)
            if op.fn is None:
                return
            ins = op.fn(eng)
            if op.dma:
                ins.then_inc(dsem[op.dsem], 16)
            elif op.signal:
                ins.then_inc(esem[eng_name][op.dsem], 1)

        def run(eng_name):
            def body(eng):
                ops = self.ops[eng_name]
                i = 0
                while i < len(ops):
                    op = ops[i]
                    if op.region is None:
                        emit_op(eng, eng_name, op)
                        i += 1
                        continue
                    j = i
                    while j < len(ops) and ops[j].region == op.region:
                        j += 1
                    grp = ops[i:j]
                    incs = {}
                    for o in grp:
                        if o.signal and not o.dma:
                            incs[o.dsem] = incs.get(o.dsem, 0) + 1
                    with eng.If_lt(self.skip_regs[eng_name], op.region + 1):
                        if eng_name == "pool":
                            for o in grp:
                                for p in o.waits:
                                    if p.dma:
                                        eng.wait_ge(dsem[p.dsem], p.dval)
                                    else:
                                        eng.wait_ge(esem[p.eng][p.dsem], p.count)
                                assert o.dma
                                eng.sem_inc(dsem[o.dsem], 16)
                        else:
                            if not incs:
                                eng.drain()
                            for ep, n in sorted(incs.items()):
                                eng.drain().then_inc(esem[eng_name][ep], n)
                    with eng.Else():
                        for o in grp:
                            emit_op(eng, eng_name, o)
                    i = j
            return body

        block.tensor(run("pe"))
        block.scalar(run("act"))
        block.vector(run("dve"))
        block.gpsimd(run("pool"))
        block.sync(run("sp"))


class T:
    __slots__ = ("ap", "b")

    def __init__(self, ap, name=""):
        self.ap = ap
        self.b = Buf(name)


class Arena:
    def __init__(self, ap):
        self.ap = ap
        self.off = 0
        self.n = ap.shape[1]
        self.top = self.n

    def mark(self):
        return self.off

    def release(self, m):
        self.off = m

    def tile(self, free, dtype, name="", top=False):
        if isinstance(free, int):
            free = (free,)
        nel = int(np.prod(free))
        sz = {F32: 4, I32: 4, BF16: 2}[dtype]
        cols = (nel * sz + 1) // 2
        cols = (cols + 15) // 16 * 16
        assert self.off + cols <= self.top, ("arena overflow", name, self.off, cols, self.top)
        if top:
            self.top -= cols
            v = self.ap[:, self.top:self.top + cols]
        else:
            v = self.ap[:, self.off:self.off + cols]
            self.off += cols
        if dtype != BF16:
            v = v.bitcast(dtype)
        v = v[:, 0:nel]
        if len(free) == 2:
            v = v.rearrange("p (a b) -> p a b", a=free[0])
        elif len(free) == 3:
            v = v.rearrange("p (a b c) -> p a b c", a=free[0], b=free[1])
        return T(v, name)


NCONST = 6 * 128 + 64 + 64 + 64 + 1


def make_consts():
    p = np.arange(128)[:, None]
    f = np.arange(128)[None, :]
    c = np.zeros((128, NCONST), np.float32)
    c[:, 0:128] = (p == f)
    c[:, 128:256] = (p < f)
    c[:, 256:384] = -(p >= f).astype(np.float32)
    c[:, 384:512] = -1.0
    c[:, 512:640] = (p // 64 == f // 64)
    c[:, 640:768] = (p <= f)
    e1 = np.arange(64)[None, :]
    c[:, 768:832] = (p < e1)
    c[:, 832:896] = (p <= e1)
    c[:, 896:960] = 512.0 * e1
    c[:, 960] = p[:, 0]
    return c


def build_program(debug=None):
    nc = bass.Bass("TRN2", target_bir_lowering=False)

    def din(name, shape, dt=F32):
        return nc.dram_tensor(name, list(shape), dt, kind="ExternalInput").ap()

    x = din("x", [S_TOK, D])
    consts = din("consts", [128, NCONST])
    w_in = din("w_in", [D, 2560])
    w_out = din("w_out", [D, D])
    n1w = din("n1w", [128, D])
    n2w = din("n2w", [128, D])
    sgunw = din("sgunw", [128, 512])
    onaw = din("onaw", [128, 512])
    qnw = din("qnw", [128, 64])
    knw = din("knw", [128, 64])
    sguwT = din("sguwT", [128, 4 * 128])
    sgubT = din("sgubT", [128, 4])
    onbw = din("onbw", [128, 4])
    rw = din("rw", [128, 8 * 32])
    rb = din("rb", [128, 32])
    need_moe = debug not in ("A", "B", "C")
    if need_moe:
        wgu = din("wgu", [NE * D, 2048])
        wd = din("wd", [NE * D, D])
        bgu = din("bgu", [NE * 128, 16])
        bd = din("bd", [NE, D])
    out = nc.dram_tensor("out", [S_TOK, D], F32, kind="ExternalOutput").ap()
    xs = nc.dram_tensor("xs_scr", [NSLOT, D], BF16, kind="Internal").ap()
    ys = nc.dram_tensor("ys_scr", [NSLOT, D], F32, kind="Internal").ap()

    S = Sched()
    st = ExitStack()
    with st:
        S.skip_regs = {"pe": st.enter_context(nc.tensor.register("nu_pe")),
                       "act": st.enter_context(nc.scalar.register("nu_act")),
                       "dve": st.enter_context(nc.vector.register("nu_dve")),
                       "pool": st.enter_context(nc.gpsimd.register("nu_pool"))}
        arena_ap = st.enter_context(nc.sbuf_tensor("arena", [128, 103000], BF16))
        A = Arena(arena_ap[:, :])
        PS = []
        PSW = []
        for i in range(4):
            pt = st.enter_context(nc.psum_tensor("psw%d" % i, [128, 1024], F32))
            PSW.append(pt[:, :].rearrange("p (c n) -> p c n", c=2))
            PS.append(T(pt[:, 0:512], "ps%d" % (2 * i)))
            PS.append(T(pt[:, 512:1024], "ps%d" % (2 * i + 1)))

        def psbf(i):
            return PS[i].ap.bitcast(BF16)

        cst = A.tile(NCONST, F32, "cst")
        S.op("sp", lambda e: e.dma_start(out=cst.ap, in_=consts[:, :]), writes=[cst.b], dma=True)
        identf = cst.ap[:, 0:128]
        cb = A.tile(6 * 128, BF16, "cb")
        S.op("dve", lambda e: e.tensor_copy(cb.ap, cst.ap[:, 0:768]), reads=[cst.b], writes=[cb.b])
        identb = cb.ap[:, 0:128]
        ltb = cb.ap[:, 128:256]
        negtri = cb.ap[:, 256:384]
        negones = cb.ap[:, 384:512]
        blkones = cb.ap[:, 512:640]
        onesb = A.tile(128, BF16, "onesb")
        S.op("dve", lambda e: e.memset(onesb.ap, 1.0), writes=[onesb.b])
        zeros = A.tile(512, BF16, "zeros")
        S.op("dve", lambda e: e.memset(zeros.ap, 0.0), writes=[zeros.b])

        def load_const(src, n, name, dt=F32):
            t = A.tile(n, dt, name)
            S.op("sp", lambda e: e.dma_start(out=t.ap, in_=src[:, :]), writes=[t.b], dma=True)
            return t

        n1w_t = load_const(n1w, D, "n1w")
        sgunw_t = load_const(sgunw, 512, "sgunw")
        onaw_t = load_const(onaw, 512, "onaw")
        qnw_t = load_const(qnw, 64, "qnw")
        knw_t = load_const(knw, 64, "knw")
        sgub_t = load_const(sgubT, 4, "sgub")
        onbw_t = load_const(onbw, 4, "onbw")
        rw_t = load_const(rw, 256, "rw")
        rb_t = load_const(rb, 32, "rb")
        S.op("dve", lambda e: e.tensor_scalar(qnw_t.ap, qnw_t.ap, 0.125, None, ALU.mult), reads=[qnw_t.b], writes=[qnw_t.b])
        sgw_f = load_const(sguwT, 512, "sgw_f")
        wmT = A.tile((4, 128), BF16, "wmT")
        S.op("dve", lambda e: e.tensor_tensor(wmT.ap, sgw_f.ap.rearrange("p (g t) -> p g t", g=4),
                                              cst.ap[:, 640:768].unsqueeze(1).to_broadcast([128, 4, 128]), ALU.mult),
             reads=[sgw_f.b, cst.b], writes=[wmT.b])

        destI = A.tile((NT, 4), I32, "destI", top=True)
        gsel = A.tile((NT, 4), F32, "gsel", top=True)
        widx = A.tile((NBLK, 8), I32, "widx", top=True)
        bidx = A.tile(NBLK, I32, "bidx", top=True)
        eidx = A.tile(NBLK, I32, "eidx", top=True)
        nuI = A.tile(1, I32, "nuI", top=True)
        top_persist = A.top
        mark0 = A.mark()
        qT = A.tile((4, S_TOK), BF16, "qT")
        kT = A.tile((4, S_TOK), BF16, "kT")
        Vt = A.tile((NT, 512), BF16, "V")
        markA = A.mark()

        zbig = A.tile(1024, BF16, "zbig")
        S.op("pool", lambda e: e.memset(zbig.ap, 0.0), writes=[zbig.b])
        xs_b = Buf("xs")
        xs_v = xs.rearrange("(n p) d -> n p d", p=128)
        xs_bufs = [Buf("xs%d" % n) for n in range(NSLOT // 128)]

        w_in_t = A.tile((8, 2560), BF16, "w_in")
        w_in_v = w_in.rearrange("(kc p) f -> p kc f", p=128)
        for kc in range(8):
            S.op("pool", (lambda kc: lambda e: e.dma_start(out=w_in_t.ap[:, kc, :], in_=w_in_v[:, kc, :]))(kc),
                 writes=[w_in_t.b], dma=True)
        w_out_t = A.tile((4, D), BF16, "w_out")
        w_out_v = w_out.rearrange("(kc p) f -> p kc f", p=128)
        for kc in range(4):
            S.op("pool", (lambda kc: lambda e: e.dma_start(out=w_out_t.ap[:, kc, :], in_=w_out_v[:, kc, :]))(kc),
                 writes=[w_out_t.b], dma=True)
        xt = [A.tile(D, F32, "xt%d" % i) for i in range(2)]
        sq = A.tile(D, BF16, "sq")
        st1 = A.tile(16, F32, "st1")
        hb = A.tile(D, BF16, "hb")
        hT = [A.tile((8, 128), BF16, "hT0")] * 2
        zt = A.tile(D, F32, "zt")
        vn = A.tile(512, BF16, "vn")
        ya = A.tile(512, F32, "ya")
        yan = A.tile(512, BF16, "yan")
        yaT = A.tile((4, 128), BF16, "yaT")
        qtmp = A.tile(512, F32, "qtmp")
        vtmp = qtmp
        qn = A.tile(512, BF16, "qn")
        kn = A.tile(512, BF16, "kn")
        x1p = A.tile(D, F32, "x1p")
        out_b = [Buf("out%d" % t) for t in range(NT)]

        epsT = A.tile(1, F32, "eps")
        S.op("dve", lambda e: e.memset(epsT.ap, EPS), writes=[epsT.b])

        def rstd_ops(ss_ap, n, ss_buf, dim):
            S.op("act", lambda e: e.activation(ss_ap, ss_ap, AF.Ln, bias=epsT.ap, scale=1.0 / dim), reads=[ss_buf, epsT.b], writes=[ss_buf])
            S.op("act", lambda e: e.activation(ss_ap, ss_ap, AF.Exp, scale=-0.5), reads=[ss_buf], writes=[ss_buf])

        def group_norm(src_ap, src_bufs, dst, groups, gdim, wt_ap, wt_buf, tmp, extra_scale_bcast_heads=False):
            n = groups * gdim
            S.op("act", lambda e: e.activation(sq.ap[:, 0:n], src_ap, AF.Square), reads=src_bufs, writes=[sq.b])
            ssv = st1.ap[:, 0:groups]
            S.op("dve", lambda e: e.tensor_reduce(ssv, sq.ap[:, 0:n].rearrange("p (g c) -> p g c", g=groups), AX.X, ALU.add),
                 reads=[sq.b], writes=[st1.b])
            rstd_ops(ssv, groups, st1.b, gdim)
            S.op("dve", lambda e: e.tensor_tensor(tmp.ap[:, 0:n].rearrange("p (g c) -> p g c", g=groups),
                                                  src_ap.rearrange("p (g c) -> p g c", g=groups),
                                                  ssv.unsqueeze(2).to_broadcast([128, groups, gdim]), ALU.mult),
                 reads=src_bufs + [st1.b], writes=[tmp.b])
            if extra_scale_bcast_heads:
                w_ap = wt_ap.unsqueeze(1).to_broadcast([128, groups, gdim])
                S.op("dve", lambda e: e.tensor_tensor(dst.ap.rearrange("p (g c) -> p g c", g=groups),
                                                      tmp.ap[:, 0:n].rearrange("p (g c) -> p g c", g=groups), w_ap, ALU.mult),
                     reads=[tmp.b, wt_buf], writes=[dst.b])
            else:
                S.op("dve", lambda e: e.tensor_tensor(dst.ap, tmp.ap[:, 0:n], wt_ap, ALU.mult), reads=[tmp.b, wt_buf], writes=[dst.b])

        for t in range(NT):
            xtt = xt[t % 2]
            hTt = hT[t % 2]
            if t == 0:
                S.op("sp", lambda e: e.dma_start(out=xt[0].ap, in_=x[0:128, :]), writes=[xt[0].b], dma=True)
            if t + 1 < NT:
                S.op("sp", (lambda t: lambda e: e.dma_start(out=xt[(t + 1) % 2].ap, in_=x[(t + 1) * 128:(t + 2) * 128, :]))(t),
                     writes=[xt[(t + 1) % 2].b], dma=True)
            for n in range(8 * t, 8 * t + 8):
                S.op("sp", (lambda n: lambda e: e.dma_start(out=xs_v[n], in_=zbig.ap))(n), reads=[zbig.b], writes=[xs_bufs[n]], dma=True)
            S.op("act", (lambda xtt: lambda e: e.activation(sq.ap, xtt.ap, AF.Square))(xtt), reads=[xtt.b], writes=[sq.b])
            S.op("dve", lambda e: e.tensor_reduce(st1.ap[:, 8:9], sq.ap, AX.X, ALU.add), reads=[sq.b], writes=[st1.b])
            rstd_ops(st1.ap[:, 8:9], 1, st1.b, D)
            S.op("dve", (lambda xtt: lambda e: e.scalar_tensor_tensor(hb.ap, xtt.ap, st1.ap[:, 8:9], n1w_t.ap, ALU.mult, ALU.mult))(xtt),
                 reads=[xtt.b, st1.b, n1w_t.b], writes=[hb.b])
            for kc in range(8):
                S.op("pe", (lambda kc: lambda e: e.transpose(psbf(0)[:, kc * 128:(kc + 1) * 128], hb.ap[:, kc * 128:(kc + 1) * 128], identb))(kc),
                     reads=[hb.b, cb.b], writes=[PS[0].b])
            S.op("act", (lambda hTt: lambda e: e.activation(hTt.ap, psbf(0).rearrange("p (a b) -> p a b", a=8), AF.Copy))(hTt),
                 reads=[PS[0].b], writes=[hTt.b])
            for cg in range(5):
                for kc in range(8):
                    S.op("pe", (lambda cg, kc, hTt: lambda e: e.matmul(PS[1 + cg].ap, hTt.ap[:, kc, :], w_in_t.ap[:, kc, cg * 512:(cg + 1) * 512],
                                                                    start=(kc == 0), stop=(kc == 7)))(cg, kc, hTt),
                         reads=[hTt.b, w_in_t.b], writes=[PS[1 + cg].b])
            S.op("act", lambda e: e.activation(zt.ap[:, 0:512], PS[1].ap, AF.Gelu_apprx_tanh), reads=[PS[1].b], writes=[zt.b])
            S.op("act", lambda e: e.activation(zt.ap[:, 512:1024], PS[2].ap, AF.Gelu_apprx_tanh), reads=[PS[2].b], writes=[zt.b])
            group_norm(zt.ap[:, 512:1024], [zt.b], vn, 4, 128, sgunw_t.ap, sgunw_t.b, vtmp)
            for g in range(4):
                S.op("pe", (lambda g: lambda e: e.matmul(PS[6].ap[:, g * 128:(g + 1) * 128], wmT.ap[:, g, :], vn.ap[:, g * 128:(g + 1) * 128],
                                                       start=True, stop=True))(g), reads=[wmT.b, vn.b], writes=[PS[6].b])
            S.op("dve", lambda e: e.tensor_tensor(ya.ap.rearrange("p (g c) -> p g c", g=4), PS[6].ap.rearrange("p (g c) -> p g c", g=4),
                                                  sgub_t.ap.unsqueeze(2).to_broadcast([128, 4, 128]), ALU.add),
                 reads=[PS[6].b, sgub_t.b], writes=[ya.b])
            S.op("dve", lambda e: e.tensor_tensor(ya.ap, ya.ap, zt.ap[:, 0:512], ALU.mult), reads=[ya.b, zt.b], writes=[ya.b])
            group_norm(ya.ap, [ya.b], yan, 4, 128, onaw_t.ap, onaw_t.b, vtmp)
            for c in range(4):
                S.op("pe", (lambda c: lambda e: e.transpose(psbf(7)[:, c * 128:(c + 1) * 128], yan.ap[:, c * 128:(c + 1) * 128], identb))(c),
                     reads=[yan.b, cb.b], writes=[PS[7].b])
            S.op("act", lambda e: e.activation(yaT.ap, psbf(7)[:, 0:512].rearrange("p (a b) -> p a b", a=4), AF.Copy),
                 reads=[PS[7].b], writes=[yaT.b])
            group_norm(PS[3].ap, [PS[3].b], qn, 8, 64, qnw_t.ap, qnw_t.b, qtmp, True)
            group_norm(PS[4].ap, [PS[4].b], kn, 8, 64, knw_t.ap, knw_t.b, qtmp, True)
            S.op("act", (lambda t: lambda e: e.activation(Vt.ap[:, t, :], PS[5].ap, AF.Copy))(t), reads=[PS[5].b], writes=[Vt.b])
            for c in range(4):
                S.op("pe", (lambda c: lambda e: e.transpose(psbf(7)[:, 512 + c * 128:512 + (c + 1) * 128], qn.ap[:, c * 128:(c + 1) * 128], identb))(c),
                     reads=[qn.b, cb.b], writes=[PS[7].b])
            S.op("act", (lambda t: lambda e: e.activation(qT.ap[:, :, t * 128:(t + 1) * 128], psbf(7)[:, 512:1024].rearrange("p (a b) -> p a b", a=4), AF.Copy))(t),
                 reads=[PS[7].b], writes=[qT.b])
            for c in range(4):
                S.op("pe", (lambda c: lambda e: e.transpose(psbf(0)[:, c * 128:(c + 1) * 128], kn.ap[:, c * 128:(c + 1) * 128], identb))(c),
                     reads=[kn.b, cb.b], writes=[PS[0].b])
            S.op("act", (lambda t: lambda e: e.activation(kT.ap[:, :, t * 128:(t + 1) * 128], psbf(0)[:, 0:512].rearrange("p (a b) -> p a b", a=4), AF.Copy))(t),
                 reads=[PS[0].b], writes=[kT.b])
            for half in range(2):
                for c in range(4):
                    S.op("pe", (lambda half, c: lambda e: e.matmul(PS[6].ap, yaT.ap[:, c, :], w_out_t.ap[:, c, half * 512:(half + 1) * 512],
                                                                 start=(c == 0), stop=(c == 3)))(half, c),
                         reads=[yaT.b, w_out_t.b], writes=[PS[6].b])
                S.op("dve", (lambda half, xtt: lambda e: e.tensor_tensor(x1p.ap[:, half * 512:(half + 1) * 512], PS[6].ap,
                                                                        xtt.ap[:, half * 512:(half + 1) * 512], ALU.add))(half, xtt),
                     reads=[PS[6].b, xtt.b], writes=[x1p.b])
            S.op("sp", (lambda t: lambda e: e.dma_start(out=out[t * 128:(t + 1) * 128, :], in_=x1p.ap))(t),
                 reads=[x1p.b], writes=[out_b[t]], dma=True)

        if debug == "A":
            dq = nc.dram_tensor("dbg_q", [128, 4 * S_TOK], BF16, kind="ExternalOutput").ap()
            dk = nc.dram_tensor("dbg_k", [128, 4 * S_TOK], BF16, kind="ExternalOutput").ap()
            dv = nc.dram_tensor("dbg_v", [128, NT * 512], BF16, kind="ExternalOutput").ap()
            S.op("sp", lambda e: e.dma_start(out=dq[:, :], in_=qT.ap.rearrange("p a b -> p (a b)")), reads=[qT.b], dma=True)
            S.op("sp", lambda e: e.dma_start(out=dk[:, :], in_=kT.ap.rearrange("p a b -> p (a b)")), reads=[kT.b], dma=True)
            S.op("sp", lambda e: e.dma_start(out=dv[:, :], in_=Vt.ap.rearrange("p a b -> p (a b)")), reads=[Vt.b], dma=True)
            S.barrier()
            S.emit(nc, st)
            return nc
        S.barrier()
        A.release(markA)
        _guard = A.tile(3072, BF16, "guard", top=True)
        yTb = A.tile((4, S_TOK), BF16, "yTb", top=True)
        qTm = A.tile((2, S_TOK), BF16, "qTm")
        Ew = A.tile((2, 512), F32, "Ew")
        SPw = [A.tile((2, 512), BF16, "SPw%d" % i) for i in range(2)]
        ATw = [A.tile((2, 512), BF16, "ATw%d" % i) for i in range(2)]
        Rw = [A.tile((2, 512), BF16, "Rw%d" % i) for i in range(2)]
        osq = A.tile(512, BF16, "osq")
        rsn = A.tile(512, F32, "rsn")
        PS_A, PS_B, PS_O, PS_SS = (PS[0], PS[1]), (PS[2], PS[3]), (PS[4], PS[5]), PS[6]
        PSA_w, PSB_w = PSW[0], PSW[1]
        ltb2 = ltb.unsqueeze(1).to_broadcast([128, 2, 128])
        S.op("pool", lambda e: e.memset(qTm.ap, 0.0), writes=[qTm.b])
        for j in range(4):
            S.op("dve", (lambda j: lambda e: e.tensor_copy(qTm.ap[0:64, 0, :], qT.ap[0:64, j, :]))(j), reads=[qT.b], writes=[qTm.b])
            S.op("pool", (lambda j: lambda e: e.tensor_copy(qTm.ap[64:128, 1, :], qT.ap[64:128, j, :]))(j), reads=[qT.b], writes=[qTm.b])

            def do_group(j, g):
                sbs = list(range(4 * g + 3, -1, -1))
                nst = len(sbs)

                def c0_of(s):
                    return max(0, sbs[s] - 4 * g) * 128

                for c in range(2):
                    S.op("pe", (lambda c: lambda e: e.matmul(PS_O[c].ap, Vt.ap[:, 0, 0:128], zeros.ap, start=True, stop=False))(c),
                         reads=[Vt.b, zeros.b], writes=[PS_O[c].b])
                S.op("pool", lambda e: e.memset(Rw[1].ap[:, :, 0:384], 0.0), writes=[Rw[1].b])
                S.op("pool", lambda e: e.memset(Rw[0].ap[:, :, 0:256], 0.0), writes=[Rw[0].b])

                def emitA(s, c):
                    sb, c0 = sbs[s], c0_of(s)
                    S.op("pe", lambda e: e.matmul(PS_A[c].ap[:, c0:512], kT.ap[:, j, sb * 128:(sb + 1) * 128],
                                                  qTm.ap[:, c, g * 512 + c0:(g + 1) * 512], start=True, stop=True),
                         reads=[kT.b, qTm.b], writes=[PS_A[c].b])

                def emit_front(s):
                    sb, c0 = sbs[s], c0_of(s)
                    sp_t = SPw[s % 2]
                    S.op("act", lambda e: e.activation(Ew.ap[:, :, c0:512], PSA_w[:, :, c0:512], AF.Exp),
                         reads=[PS_A[0].b, PS_A[1].b], writes=[Ew.b])
                    if s + 1 < nst:
                        for c in range(2):
                            emitA(s + 1, c)
                    S.op("act", lambda e: e.activation(sp_t.ap[:, :, c0:512], Ew.ap[:, :, c0:512], AF.Ln, bias=1.0), reads=[Ew.b], writes=[sp_t.b])
                    if sb >= 4 * g:
                        S.op("dve", lambda e: e.tensor_tensor(sp_t.ap[:, :, c0:c0 + 128], sp_t.ap[:, :, c0:c0 + 128], ltb2, ALU.mult),
                             reads=[sp_t.b, cb.b], writes=[sp_t.b])

                def emit_B(s):
                    sb, c0 = sbs[s], c0_of(s)
                    sp_t = SPw[s % 2]
                    for c in range(2):
                        S.op("pe", (lambda c: lambda e: e.matmul(PS_B[c].ap[:, c0:512], kT.ap[:, j, sb * 128:(sb + 1) * 128],
                                                               qTm.ap[:, c, g * 512 + c0:(g + 1) * 512], start=True, stop=False))(c),
                             reads=[kT.b, qTm.b], writes=[PS_B[c].b])
                        S.op("pe", (lambda c: lambda e: e.matmul(PS_B[c].ap[:, c0:512], negtri, sp_t.ap[:, c, c0:512], start=False, stop=(s == 0)))(c),
                             reads=[cb.b, sp_t.b], writes=[PS_B[c].b])
                        if s > 0:
                            S.op("pe", (lambda c: lambda e: e.matmul(PS_B[c].ap[:, c0:512], negones, Rw[s % 2].ap[:, c, c0:512], start=False, stop=True))(c),
                                 reads=[cb.b, Rw[s % 2].b], writes=[PS_B[c].b])
                    if s + 1 < nst:
                        r_n = Rw[(s + 1) % 2]
                        if s == 0:
                            S.op("pool", lambda e: e.tensor_copy(r_n.ap[:, :, c0:512], sp_t.ap[:, :, c0:512]), reads=[sp_t.b], writes=[r_n.b])
                        else:
                            r_c = Rw[s % 2]
                            S.op("dve", lambda e: e.tensor_tensor(r_n.ap[:, :, c0:512], r_c.ap[:, :, c0:512], sp_t.ap[:, :, c0:512], ALU.add),
                                 reads=[r_c.b, sp_t.b], writes=[r_n.b])

                def emit_back(s):
                    sb, c0 = sbs[s], c0_of(s)
                    at_t = ATw[s % 2]
                    S.op("act", lambda e: e.activation(at_t.ap[:, :, c0:512], PSB_w[:, :, c0:512], AF.Exp),
                         reads=[PS_B[0].b, PS_B[1].b], writes=[at_t.b])
                    if sb >= 4 * g:
                        S.op("dve", lambda e: e.tensor_tensor(at_t.ap[:, :, c0:c0 + 128], at_t.ap[:, :, c0:c0 + 128], ltb2, ALU.mult),
                             reads=[at_t.b, cb.b], writes=[at_t.b])
                    for c in range(2):
                        S.op("pe", (lambda c: lambda e: e.matmul(PS_O[c].ap[:, c0:512], Vt.ap[:, sb, j * 128:(j + 1) * 128], at_t.ap[:, c, c0:512],
                                                               start=False, stop=(s == nst - 1)))(c),
                             reads=[Vt.b, at_t.b], writes=[PS_O[c].b])

                for c in range(2):
                    emitA(0, c)
                for s in range(nst):
                    emit_front(s)
                    if s >= 1:
                        emit_back(s - 1)
                    emit_B(s)
                emit_back(nst - 1)
                for c in range(2):
                    S.op("act", (lambda c: lambda e: e.activation(osq.ap[c * 64:(c + 1) * 64, :], PS_O[c].ap[c * 64:(c + 1) * 64, :], AF.Square))(c),
                         reads=[PS_O[c].b], writes=[osq.b])
                S.op("pe", lambda e: e.matmul(PS_SS.ap, blkones, osq.ap, start=True, stop=True), reads=[cb.b, osq.b], writes=[PS_SS.b])
                S.op("act", lambda e: e.activation(rsn.ap, PS_SS.ap, AF.Ln, bias=epsT.ap, scale=1.0 / 64), reads=[PS_SS.b, epsT.b], writes=[rsn.b])
                S.op("act", lambda e: e.activation(rsn.ap, rsn.ap, AF.Exp, scale=-0.5), reads=[rsn.b], writes=[rsn.b])
                for c in range(2):
                    S.op("dve", (lambda c, j, g: lambda e: e.scalar_tensor_tensor(
                        yTb.ap[c * 64:(c + 1) * 64, j, g * 512:(g + 1) * 512], PS_O[c].ap[c * 64:(c + 1) * 64, :],
                        onbw_t.ap[c * 64:(c + 1) * 64, j:j + 1], rsn.ap[c * 64:(c + 1) * 64, :], ALU.mult, ALU.mult))(c, j, g),
                        reads=[PS_O[c].b, onbw_t.b, rsn.b], writes=[yTb.b])

            for g in range(8):
                do_group(j, g)

        if debug == "B":
            dy = nc.dram_tensor("dbg_y", [128, 4 * S_TOK], BF16, kind="ExternalOutput").ap()
            dk = nc.dram_tensor("dbg_k", [128, 4 * S_TOK], BF16, kind="ExternalOutput").ap()
            dv = nc.dram_tensor("dbg_v", [128, NT * 512], BF16, kind="ExternalOutput").ap()
            dq = nc.dram_tensor("dbg_q", [128, 2 * S_TOK], BF16, kind="ExternalOutput").ap()
            S.op("sp", lambda e: e.dma_start(out=dy[:, :], in_=yTb.ap.rearrange("p a b -> p (a b)")), reads=[yTb.b], dma=True)
            S.op("sp", lambda e: e.dma_start(out=dk[:, :], in_=kT.ap.rearrange("p a b -> p (a b)")), reads=[kT.b], dma=True)
            S.op("sp", lambda e: e.dma_start(out=dv[:, :], in_=Vt.ap.rearrange("p a b -> p (a b)")), reads=[Vt.b], dma=True)
            S.op("sp", lambda e: e.dma_start(out=dq[:, :], in_=qTm.ap.rearrange("p a b -> p (a b)")), reads=[qTm.b], dma=True)
            S.barrier()
            S.emit(nc, st)
            return nc

        S.barrier()
        A.release(mark0)
        h2s = A.tile((NT, D), BF16, "h2s")
        w_outB = A.tile((4, D), BF16, "w_outB")
        for kc in range(4):
            S.op("pool", (lambda kc: lambda e: e.dma_start(out=w_outB.ap[:, kc, :], in_=w_out_v[:, 4 + kc, :]))(kc), writes=[w_outB.b], dma=True)
        n2w_t = load_const(n2w, D, "n2w")
        maskS = A.tile((NT, 32), F32, "maskS")
        rankS = A.tile((NT, 32), F32, "rankS")
        gateS = A.tile((NT, 32), F32, "gateS")
        cumf = A.tile(32, F32, "cumf")
        cumb = A.tile(32, BF16, "cumb")
        maskb_l = [A.tile(32, BF16, "maskb%d" % i) for i in range(2)]
        x1t = [A.tile(D, F32, "x1t%d" % i) for i in range(2)]
        h2f_l = [A.tile(D, F32, "h2f%d" % i) for i in range(2)]
        sq2_l = [A.tile(D, F32, "sq2%d" % i) for i in range(2)]
        h2T_l = [A.tile((8, 128), F32, "h2T%d" % i) for i in range(2)]
        lg_l = [A.tile(32, F32, "lg%d" % i) for i in range(2)]
        ex_l = [A.tile(32, F32, "ex%d" % i) for i in range(2)]
        mx8_l = [A.tile(8, F32, "mx8%d" % i) for i in range(2)]
        sm_l = [A.tile(16, F32, "sm%d" % i) for i in range(2)]
        sq2, sm = sq2_l[0], sm_l[0]
        S.op("dve", lambda e: e.memset(cumf.ap, 0.0), writes=[cumf.b])

        def tileC1(t):
            xx = x1t[t % 2]
            pp = t % 2
            pb = 4 * pp
            h2f, sq2, h2T, lg, ex, mx8, sm, maskb = h2f_l[pp], sq2_l[pp], h2T_l[pp], lg_l[pp], ex_l[pp], mx8_l[pp], sm_l[pp], maskb_l[pp]
            S.op("sp", (lambda t, xx: lambda e: e.dma_start(out=xx.ap, in_=out[t * 128:(t + 1) * 128, :]))(t, xx),
                 reads=[out_b[t]], writes=[xx.b], dma=True)
            for half in range(2):
                for c in range(4):
                    S.op("pe", (lambda half, c, t: lambda e: e.matmul(PS[pb + half].ap, yTb.ap[:, c, t * 128:(t + 1) * 128],
                                                                    w_outB.ap[:, c, half * 512:(half + 1) * 512], start=(c == 0), stop=(c == 3)))(half, c, t),
                         reads=[yTb.b, w_outB.b], writes=[PS[pb + half].b])
                S.op("dve", (lambda half, xx: lambda e: e.tensor_tensor(xx.ap[:, half * 512:(half + 1) * 512], PS[pb + half].ap,
                                                                       xx.ap[:, half * 512:(half + 1) * 512], ALU.add))(half, xx),
                     reads=[PS[pb + half].b, xx.b], writes=[xx.b])
            S.op("sp", (lambda t, xx: lambda e: e.dma_start(out=out[t * 128:(t + 1) * 128, :], in_=xx.ap))(t, xx),
                 reads=[xx.b], writes=[out_b[t]], dma=True)
            S.op("act", (lambda xx: lambda e: e.activation(sq2.ap, xx.ap, AF.Square))(xx), reads=[xx.b], writes=[sq2.b])
            S.op("dve", lambda e: e.tensor_reduce(sm.ap[:, 0:1], sq2.ap, AX.X, ALU.add), reads=[sq2.b], writes=[sm.b])
            rstd_ops(sm.ap[:, 0:1], 1, sm.b, D)
            S.op("dve", (lambda xx: lambda e: e.scalar_tensor_tensor(h2f.ap, xx.ap, sm.ap[:, 0:1], n2w_t.ap, ALU.mult, ALU.mult))(xx),
                 reads=[xx.b, sm.b, n2w_t.b], writes=[h2f.b])
            S.op("pool", (lambda t: lambda e: e.tensor_copy(h2s.ap[:, t, :], h2f.ap))(t), reads=[h2f.b], writes=[h2s.b])
            for kc in range(8):
                S.op("pe", (lambda kc: lambda e: e.transpose(PS[pb + 2 + kc // 4].ap[:, (kc % 4) * 128:(kc % 4 + 1) * 128],
                                                              h2f.ap[:, kc * 128:(kc + 1) * 128], identf))(kc),
                     reads=[h2f.b, cst.b], writes=[PS[pb + 2 + kc // 4].b])
            for hh in range(2):
                S.op("act", (lambda hh: lambda e: e.activation(h2T.ap[:, hh * 4:(hh + 1) * 4, :], PS[pb + 2 + hh].ap.rearrange("p (a b) -> p a b", a=4), AF.Copy))(hh),
                     reads=[PS[pb + 2 + hh].b], writes=[h2T.b])
            for kc in range(8):
                S.op("pe", (lambda kc: lambda e: e.matmul(PS[pb].ap[:, 0:32], h2T.ap[:, kc, :], rw_t.ap[:, kc * 32:(kc + 1) * 32],
                                                        start=(kc == 0), stop=(kc == 7)))(kc),
                     reads=[h2T.b, rw_t.b], writes=[PS[pb].b])

        def tileC2(t):
            xx = x1t[t % 2]
            pp = t % 2
            pb = 4 * pp
            h2f, sq2, h2T, lg, ex, mx8, sm, maskb = h2f_l[pp], sq2_l[pp], h2T_l[pp], lg_l[pp], ex_l[pp], mx8_l[pp], sm_l[pp], maskb_l[pp]
            S.op("dve", lambda e: e.tensor_tensor(lg.ap, PS[pb].ap[:, 0:32], rb_t.ap, ALU.add), reads=[PS[pb].b, rb_t.b], writes=[lg.b])
            S.op("dve", lambda e: e.max(mx8.ap, lg.ap), reads=[lg.b], writes=[mx8.b])
            S.op("dve", (lambda t: lambda e: e.tensor_scalar(maskS.ap[:, t, :], lg.ap, mx8.ap[:, 3:4], None, ALU.is_ge))(t),
                 reads=[lg.b, mx8.b], writes=[maskS.b])
            S.op("dve", lambda e: e.tensor_scalar(sm.ap[:, 1:2], mx8.ap[:, 0:1], -1.0, None, ALU.mult), reads=[mx8.b], writes=[sm.b])
            S.op("act", lambda e: e.activation(ex.ap, lg.ap, AF.Exp, bias=sm.ap[:, 1:2]), reads=[lg.b, sm.b], writes=[ex.b])
            S.op("dve", (lambda t: lambda e: e.tensor_tensor(ex.ap, ex.ap, maskS.ap[:, t, :], ALU.mult))(t), reads=[ex.b, maskS.b], writes=[ex.b])
            S.op("dve", lambda e: e.tensor_reduce(sm.ap[:, 2:3], ex.ap, AX.X, ALU.add), reads=[ex.b], writes=[sm.b])
            S.op("dve", lambda e: e.reciprocal(sm.ap[:, 2:3], sm.ap[:, 2:3]), reads=[sm.b], writes=[sm.b])
            S.op("dve", (lambda t: lambda e: e.tensor_scalar(gateS.ap[:, t, :], ex.ap, sm.ap[:, 2:3], None, ALU.mult))(t),
                 reads=[ex.b, sm.b], writes=[gateS.b])
            S.op("dve", (lambda t: lambda e: e.tensor_copy(maskb.ap, maskS.ap[:, t, :]))(t), reads=[maskS.b], writes=[maskb.b])
            S.op("pe", lambda e: e.matmul(PS[pb + 1].ap[:, 0:32], ltb, maskb.ap, start=True, stop=(t == 0)), reads=[cb.b, maskb.b], writes=[PS[pb + 1].b])
            if t > 0:
                S.op("pe", lambda e: e.matmul(PS[pb + 1].ap[:, 0:32], onesb.ap, cumb.ap, start=False, stop=True), reads=[onesb.b, cumb.b], writes=[PS[pb + 1].b])
            S.op("act", (lambda t: lambda e: e.activation(rankS.ap[:, t, :], PS[pb + 1].ap[:, 0:32], AF.Copy))(t), reads=[PS[pb + 1].b], writes=[rankS.b])
            S.op("dve", (lambda t: lambda e: e.tensor_tensor(cumf.ap, cumf.ap, maskS.ap[:, t, :], ALU.add))(t), reads=[cumf.b, maskS.b], writes=[cumf.b])
            S.op("dve", lambda e: e.tensor_copy(cumb.ap, cumf.ap), reads=[cumf.b], writes=[cumb.b])
        tileC1(0)
        for t in range(NT):
            if t + 1 < NT:
                tileC1(t + 1)
            tileC2(t)
        cnt = A.tile(128, F32, "cnt")
        pst = A.tile(64, F32, "pst")
        big = A.tile((NBLK, 32), F32, "big")
        ebf = A.tile(NBLK, F32, "ebf")
        wf = A.tile((NBLK, 8), F32, "wf")
        S.op("pe", lambda e: e.matmul(PS[6].ap[0:32, 0:128], cumb.ap, onesb.ap, start=True, stop=True), reads=[cumb.b, onesb.b], writes=[PS[6].b])
        S.op("dve", lambda e: e.tensor_copy(cnt.ap[0:32, :], PS[6].ap[0:32, 0:128]), reads=[PS[6].b], writes=[cnt.b])
        S.op("dve", lambda e: e.tensor_tensor(sq2.ap[0:32, 0:64], cnt.ap[0:32, 0:64], cst.ap[0:32, 896:960], ALU.is_gt), reads=[cnt.b, cst.b], writes=[sq2.b])
        S.op("dve", lambda e: e.tensor_reduce(sm.ap[0:32, 4:5], sq2.ap[0:32, 0:64], AX.X, ALU.add), reads=[sq2.b], writes=[sm.b])
        S.op("dve", lambda e: e.tensor_scalar(cnt.ap[0:32, :], cst.ap[0:32, 384:512], sm.ap[0:32, 4:5], -512.0, ALU.mult, ALU.mult), reads=[cst.b, sm.b], writes=[cnt.b])
        S.op("pe", lambda e: e.matmul(PS[7].ap[:, 0:32], cnt.ap[0:32, :], cst.ap[0:32, 768:800], start=True, stop=True), reads=[cnt.b, cst.b], writes=[PS[7].b])
        S.op("pe", lambda e: e.matmul(PS[7].ap[:, 32:64], cnt.ap[0:32, :], cst.ap[0:32, 832:864], start=True, stop=True), reads=[cnt.b, cst.b], writes=[PS[7].b])
        S.op("dve", lambda e: e.tensor_copy(pst.ap, PS[7].ap[:, 0:64]), reads=[PS[7].b], writes=[pst.b])
        S.op("dve", lambda e: e.tensor_tensor(big.ap, pst.ap[:, 32:64].unsqueeze(1).to_broadcast([128, NBLK, 32]),
                                              cst.ap[:, 896:960].unsqueeze(2).to_broadcast([128, NBLK, 32]), ALU.is_le),
             reads=[pst.b, cst.b], writes=[big.b])
        S.op("dve", lambda e: e.tensor_reduce(ebf.ap, big.ap, AX.X, ALU.add), reads=[big.b], writes=[ebf.b])
        S.op("dve", lambda e: e.tensor_scalar(ebf.ap, ebf.ap, 31.0, None, ALU.min), reads=[ebf.b], writes=[ebf.b])
        nuf = A.tile(1, F32, "nuf")
        S.op("dve", lambda e: e.tensor_scalar(nuf.ap, pst.ap[:, 63:64], 1.0 / 512.0, None, ALU.mult), reads=[pst.b], writes=[nuf.b])
        S.op("dve", lambda e: e.tensor_copy(nuI.ap, nuf.ap), reads=[nuf.b], writes=[nuI.b])
        S.op("dve", lambda e: e.tensor_copy(eidx.ap, ebf.ap), reads=[ebf.b], writes=[eidx.b])
        S.op("dve", lambda e: e.tensor_scalar(sq2.ap[:, 0:NBLK], ebf.ap, 128.0, cst.ap[:, 960:961], ALU.mult, ALU.add), reads=[ebf.b, cst.b], writes=[sq2.b])
        S.op("dve", lambda e: e.tensor_copy(bidx.ap, sq2.ap[:, 0:NBLK]), reads=[sq2.b], writes=[bidx.b])
        S.op("dve", lambda e: e.tensor_scalar(sq2.ap[:, 0:NBLK], ebf.ap, 1024.0, cst.ap[:, 960:961], ALU.mult, ALU.add), reads=[ebf.b, cst.b], writes=[sq2.b])
        for kc in range(8):
            S.op("dve", (lambda kc: lambda e: e.tensor_scalar(wf.ap[:, :, kc], sq2.ap[:, 0:NBLK], float(kc * 128), None, ALU.add))(kc),
                 reads=[sq2.b], writes=[wf.b])
        S.op("dve", lambda e: e.tensor_copy(widx.ap, wf.ap), reads=[wf.b], writes=[widx.b])
        S.op("dve", lambda e: e.tensor_scalar(pst.ap[:, 0:32], pst.ap[:, 0:32], 1.0, None, ALU.add), reads=[pst.b], writes=[pst.b])
        val = A.tile(32, F32, "val")
        eqt = A.tile(32, F32, "eqt")
        top8 = A.tile(8, F32, "top8")
        dsf = A.tile(4, F32, "dsf")
        scat_ops = []
        for t in range(NT):
            S.op("dve", (lambda t: lambda e: e.tensor_tensor(val.ap, rankS.ap[:, t, :], pst.ap[:, 0:32], ALU.add))(t), reads=[rankS.b, pst.b], writes=[val.b])
            S.op("dve", (lambda t: lambda e: e.tensor_tensor(val.ap, val.ap, maskS.ap[:, t, :], ALU.mult))(t), reads=[val.b, maskS.b], writes=[val.b])
            S.op("dve", lambda e: e.max(top8.ap, val.ap), reads=[val.b], writes=[top8.b])
            for k in range(4):
                S.op("dve", (lambda k: lambda e: e.tensor_scalar(eqt.ap, val.ap, top8.ap[:, k:k + 1], None, ALU.is_equal))(k), reads=[val.b, top8.b], writes=[eqt.b])
                S.op("dve", (lambda t: lambda e: e.tensor_tensor(eqt.ap, eqt.ap, gateS.ap[:, t, :], ALU.mult))(t), reads=[eqt.b, gateS.b], writes=[eqt.b])
                S.op("dve", (lambda t, k: lambda e: e.tensor_reduce(gsel.ap[:, t, k:k + 1], eqt.ap, AX.X, ALU.add))(t, k), reads=[eqt.b], writes=[gsel.b])
            S.op("dve", lambda e: e.tensor_scalar(dsf.ap, top8.ap[:, 0:4], -1.0, None, ALU.add), reads=[top8.b], writes=[dsf.b])
            S.op("dve", (lambda t: lambda e: e.tensor_copy(destI.ap[:, t, :], dsf.ap))(t), reads=[dsf.b], writes=[destI.b])
            for k in range(4):
                scat_ops.append(S.op("pool", (lambda t, k: lambda e: e.indirect_dma_start(
                    out=xs[:, :], out_offset=bass.IndirectOffsetOnAxis(ap=destI.ap[:, t, k:k + 1], axis=0),
                    in_=h2s.ap[:, t, :], in_offset=None))(t, k), reads=[h2s.b, destI.b], dma=True))

        if debug == "C":
            dd = nc.dram_tensor("dbg_dest", [128, NT * 4], I32, kind="ExternalOutput").ap()
            dg = nc.dram_tensor("dbg_gsel", [128, NT * 4], F32, kind="ExternalOutput").ap()
            de = nc.dram_tensor("dbg_eidx", [128, NBLK], I32, kind="ExternalOutput").ap()
            dw = nc.dram_tensor("dbg_widx", [128, NBLK * 8], I32, kind="ExternalOutput").ap()
            dxs = nc.dram_tensor("dbg_xs", [2048, D], BF16, kind="ExternalOutput").ap()
            S.op("sp", lambda e: e.dma_start(out=dd[:, :], in_=destI.ap.rearrange("p a b -> p (a b)")), reads=[destI.b], dma=True)
            S.op("sp", lambda e: e.dma_start(out=dg[:, :], in_=gsel.ap.rearrange("p a b -> p (a b)")), reads=[gsel.b], dma=True)
            S.op("sp", lambda e: e.dma_start(out=de[:, :], in_=eidx.ap), reads=[eidx.b], dma=True)
            S.op("sp", lambda e: e.dma_start(out=dw[:, :], in_=widx.ap.rearrange("p a b -> p (a b)")), reads=[widx.b], dma=True)
            S.barrier()
            S.op("sp", lambda e: e.dma_start(out=dxs[:, :], in_=xs[0:2048, :]), dma=True)
            S.barrier()
            S.emit(nc, st)
            return nc

        S.barrier()
        A.release(mark0)
        A.top = top_persist
        Wg = [A.tile((8, 2048), BF16, "Wg%d" % i) for i in range(2)]
        Wd = [A.tile((8, D), BF16, "Wd%d" % i) for i in range(2)]
        Wg_b = [[Buf() for _ in range(8)] for _ in range(2)]
        Wd_b = [[Buf() for _ in range(8)] for _ in range(2)]
        xin = [A.tile((4, D), BF16, "xin%d" % i) for i in range(2)]
        xTt = A.tile((8, 512), BF16, "xT")
        actT = A.tile((8, 512), BF16, "actT")
        bgu_t = [A.tile(16, F32, "bgu%d" % i) for i in range(2)]
        bd_t = [A.tile(D, F32, "bd%d" % i) for i in range(2)]
        g1 = [A.tile(512, F32, "g1%d" % i) for i in range(2)]
        sg = [A.tile(512, F32, "sg%d" % i) for i in range(2)]
        u1 = [A.tile(512, F32, "u1%d" % i) for i in range(2)]
        yo = [A.tile(D, F32, "yo%d" % i) for i in range(2)]
        ys_b = Buf("ys")
        PS_G, PS_U, PS_Y = (PS[1], PS[2]), (PS[3], PS[4]), (PS[5], PS[6])

        def load_block(b):
            bf = b % 2
            S.region_pool = b if b >= 32 else None
            S.op("sp", lambda e: e.dma_start(out=xin[bf].ap, in_=xs[b * BLK:(b + 1) * BLK, :].rearrange("(j p) d -> p j d", p=128)),
                 writes=[xin[bf].b], dma=True)
            S.op("pool", lambda e: e.indirect_dma_start(out=bgu_t[bf].ap, out_offset=None, in_=bgu[:, :],
                                                        in_offset=bass.IndirectOffsetOnAxis(ap=bidx.ap[:, b:b + 1], axis=0)),
                 reads=[bidx.b], writes=[bgu_t[bf].b], dma=True)
            S.op("pool", lambda e: e.indirect_dma_start(out=bd_t[bf].ap, out_offset=None, in_=bd[:, :],
                                                        in_offset=bass.IndirectOffsetOnAxis(ap=eidx.ap[:, b:b + 1], axis=0)),
                 reads=[eidx.b], writes=[bd_t[bf].b], dma=True)
            for kc in range(8):
                S.op("pool", (lambda kc: lambda e: e.indirect_dma_start(out=Wg[bf].ap[:, kc, :], out_offset=None, in_=wgu[:, :],
                                                                        in_offset=bass.IndirectOffsetOnAxis(ap=widx.ap[:, b, kc:kc + 1], axis=0)))(kc),
                     reads=[widx.b], writes=[Wg_b[bf][kc]], dma=True)
            for kc in range(8):
                S.op("pool", (lambda kc: lambda e: e.indirect_dma_start(out=Wd[bf].ap[:, kc, :], out_offset=None, in_=wd[:, :],
                                                                        in_offset=bass.IndirectOffsetOnAxis(ap=widx.ap[:, b, kc:kc + 1], axis=0)))(kc),
                     reads=[widx.b], writes=[Wd_b[bf][kc]], dma=True)

        for en in ("pe", "act", "dve", "pool"):
            S.op(en, (lambda en: lambda e: e.reg_load(S.skip_regs[en], nuI.ap[0:1, 0:1]))(en), reads=[nuI.b])
        load_block(0)

        def do_block(b):
            bf = b % 2
            S.region = b if b >= 32 else None
            if b + 1 < NBLK:
                load_block(b + 1)
            for kp in range(4):
                for kk in range(2):
                    kc = kp * 2 + kk
                    for jj in range(4):
                        S.op("pe", (lambda kc, kk, jj: lambda e: e.transpose(psbf(0)[:, kk * 512 + jj * 128: kk * 512 + (jj + 1) * 128],
                                                                              xin[bf].ap[:, jj, kc * 128:(kc + 1) * 128], identb))(kc, kk, jj),
                             reads=[xin[bf].b, cb.b], writes=[PS[0].b])
                S.op("act", (lambda kp: lambda e: e.activation(xTt.ap[:, kp * 2:kp * 2 + 2, :], psbf(0).rearrange("p (a b) -> p a b", a=2), AF.Copy))(kp),
                     reads=[PS[0].b], writes=[xTt.b])
            for m in range(8):
                pg, pu = PS_G[m % 2], PS_U[m % 2]
                g1t, sgt, u1t = g1[m % 2], sg[m % 2], u1[m % 2]
                for kc in range(8):
                    S.op("pe", (lambda m, kc, pg: lambda e: e.matmul(pg.ap, Wg[bf].ap[:, kc, m * 128:(m + 1) * 128], xTt.ap[:, kc, :],
                                                                   start=(kc == 0), stop=(kc == 7)))(m, kc, pg),
                         reads=[Wg_b[bf][kc], xTt.b], writes=[pg.b])
                for kc in range(8):
                    S.op("pe", (lambda m, kc, pu: lambda e: e.matmul(pu.ap, Wg[bf].ap[:, kc, 1024 + m * 128:1024 + (m + 1) * 128], xTt.ap[:, kc, :],
                                                                   start=(kc == 0), stop=(kc == 7)))(m, kc, pu),
                         reads=[Wg_b[bf][kc], xTt.b], writes=[pu.b])
                S.op("dve", (lambda m, pg, g1t: lambda e: e.tensor_scalar(g1t.ap, pg.ap, bgu_t[bf].ap[:, m:m + 1], 7.0, ALU.add, ALU.min))(m, pg, g1t),
                     reads=[pg.b, bgu_t[bf].b], writes=[g1t.b])
                S.op("act", (lambda g1t, sgt: lambda e: e.activation(sgt.ap, g1t.ap, AF.Sigmoid, scale=1.702))(g1t, sgt), reads=[g1t.b], writes=[sgt.b])
                S.op("dve", (lambda m, pu, u1t: lambda e: e.tensor_scalar(u1t.ap, pu.ap, bgu_t[bf].ap[:, 8 + m:9 + m], 7.0, ALU.add, ALU.min))(m, pu, u1t),
                     reads=[pu.b, bgu_t[bf].b], writes=[u1t.b])
                S.op("dve", (lambda u1t: lambda e: e.tensor_scalar(u1t.ap, u1t.ap, -7.0, 1.0, ALU.max, ALU.add))(u1t), reads=[u1t.b], writes=[u1t.b])
                S.op("dve", (lambda g1t, sgt: lambda e: e.tensor_tensor(g1t.ap, g1t.ap, sgt.ap, ALU.mult))(g1t, sgt), reads=[g1t.b, sgt.b], writes=[g1t.b])
                S.op("dve", (lambda m, g1t, u1t: lambda e: e.tensor_tensor(actT.ap[:, m, :], u1t.ap, g1t.ap, ALU.mult))(m, g1t, u1t),
                     reads=[u1t.b, g1t.b], writes=[actT.b])
            for jj in range(4):
                yot = yo[jj % 2]
                for half in range(2):
                    py = PS_Y[half]
                    for m in range(8):
                        S.op("pe", (lambda jj, half, m, py: lambda e: e.matmul(py.ap, actT.ap[:, m, jj * 128:(jj + 1) * 128],
                                                                             Wd[bf].ap[:, m, half * 512:(half + 1) * 512],
                                                                             start=(m == 0), stop=(m == 7)))(jj, half, m, py),
                             reads=[actT.b, Wd_b[bf][m]], writes=[py.b])
                    S.op("dve", (lambda half, py, yot: lambda e: e.tensor_tensor(yot.ap[:, half * 512:(half + 1) * 512], py.ap,
                                                                                bd_t[bf].ap[:, half * 512:(half + 1) * 512], ALU.add))(half, py, yot),
                         reads=[py.b, bd_t[bf].b], writes=[yot.b])
                S.op("sp", (lambda jj, yot: lambda e: e.dma_start(out=ys[b * BLK + jj * 128:b * BLK + (jj + 1) * 128, :], in_=yot.ap))(jj, yot),
                     reads=[yot.b], dma=True)

        for b in range(NBLK):
            do_block(b)
            S.region = None
        S.region_pool = None

        S.barrier(force=True)
        A.release(mark0)
        Yg = [A.tile((4, D), F32, "Yg%d" % i) for i in range(4)]
        xo = [A.tile(D, F32, "xo%d" % i) for i in range(4)]
        Yg_b = [[Buf() for _ in range(4)] for _ in range(4)]
        for t in range(NT):
            yg, xx = Yg[t % 4], xo[t % 4]
            S.op("sp", (lambda t, xx: lambda e: e.dma_start(out=xx.ap, in_=out[t * 128:(t + 1) * 128, :]))(t, xx),
                 reads=[out_b[t]], writes=[xx.b], dma=True)
            for k in range(4):
                S.op("pool", (lambda t, k, yg: lambda e: e.indirect_dma_start(out=yg.ap[:, k, :], out_offset=None, in_=ys[:, :],
                                                                              in_offset=bass.IndirectOffsetOnAxis(ap=destI.ap[:, t, k:k + 1], axis=0)))(t, k, yg),
                     reads=[destI.b], writes=[Yg_b[t % 4][k]], dma=True)
            for k in range(4):
                S.op("dve", (lambda t, k, yg, xx: lambda e: e.scalar_tensor_tensor(xx.ap, yg.ap[:, k, :], gsel.ap[:, t, k:k + 1], xx.ap, ALU.mult, ALU.add))(t, k, yg, xx),
                     reads=[Yg_b[t % 4][k], gsel.b, xx.b], writes=[xx.b])
            S.op("sp", (lambda t, xx: lambda e: e.dma_start(out=out[t * 128:(t + 1) * 128, :], in_=xx.ap))(t, xx),
                 reads=[xx.b], writes=[out_b[t]], dma=True)
        S.barrier()
        S.emit(nc, st)
    return nc


def _rep(v, n=128):
    return np.ascontiguousarray(np.broadcast_to(np.asarray(v, np.float32).reshape(1, -1), (n, v.size)))


def prep_shared(inp):
    f = lambda a: np.ascontiguousarray(np.asarray(a, np.float32))
    sh = {}
    sh["consts"] = make_consts()
    sh["w_in"] = f(inp["w_in"][0])
    sh["w_out"] = f(inp["w_out"][0])
    sh["n1w"] = _rep(inp["norm1_w"][0])
    sh["n2w"] = _rep(inp["norm2_w"][0])
    sh["sgunw"] = _rep(inp["sgu_norm_w"][0])
    sh["onaw"] = _rep(inp["out_norm_a_w"][0])
    sh["qnw"] = _rep(inp["q_norm_w"][0])
    sh["knw"] = _rep(inp["k_norm_w"][0])
    sh["sguwT"] = f(np.asarray(inp["sgu_w"][0]).transpose(2, 0, 1).reshape(128, 512))
    sh["sgubT"] = f(np.asarray(inp["sgu_b"][0]).T)
    sh["onbw"] = f(np.asarray(inp["out_norm_b_w"][0]).reshape(4, 128).T)
    sh["rw"] = f(np.asarray(inp["router_w"][0]).reshape(8, 128, 32).transpose(1, 0, 2).reshape(128, 256))
    sh["rb"] = _rep(inp["router_b"][0])
    wg = np.asarray(inp["w_gate_up"][0], np.float32).reshape(NE, D, D, 2)
    sh["wgu"] = np.ascontiguousarray(wg.transpose(0, 1, 3, 2).reshape(NE * D, 2048))
    sh["wd"] = f(np.asarray(inp["w_down"][0]).reshape(NE * D, D))
    if os.environ.get("KDEBUG") in ("A", "B", "C"):
        del sh["wgu"], sh["wd"]
        return sh
    b = np.asarray(inp["b_gate_up"][0], np.float32).reshape(NE, D, 2)
    bg = b[:, :, 0].reshape(NE, 8, 128).transpose(0, 2, 1)
    bu = b[:, :, 1].reshape(NE, 8, 128).transpose(0, 2, 1)
    sh["bgu"] = np.ascontiguousarray(np.concatenate([bg, bu], axis=2).reshape(NE * 128, 16))
    sh["bd"] = f(inp["b_down"][0])
    return sh


def kernel(**inputs):
    debug = os.environ.get("KDEBUG") or None
    nc = build_program(debug)
    sh = prep_shared(inputs)
    xfull = np.asarray(inputs["x"], np.float32)
    ncores = 8 if debug is None else int(os.environ.get("KCORES", "1"))
    in_maps = []
    for c in range(ncores):
        m = dict(sh)
        m["x"] = np.ascontiguousarray(xfull[c])
        in_maps.append(m)
    res = run_bass_kernel_spmd(nc, in_maps, core_ids=list(range(ncores)))
    outs = [np.asarray(r["out"]) for r in res.results]
    if debug is not None:
        return res.results
    return np.stack(outs, axis=0).astype(np.float32)
```
